# Optimizing a Trainium2 kernel written in Bass

```python
import jax
import jax.numpy as jnp
from jax import lax
import numpy as np


D_MODEL = 2048
BATCH = 1
SEQ = 16384
DEPTH = 2

GRID_W = 64
CTX_LEN = 256
N_MIXERS = 2
EPS = 1e-6
ROPE_THETA = 10000.0
Q_BLOCK = 128

MLA_HEADS = 16
Q_LORA = 512
KV_LORA = 512
QK_NOPE = 128
QK_ROPE = 64
QK_HEAD = QK_NOPE + QK_ROPE
V_DIM = 128

NA_HEADS = 16
NA_HEAD_DIM = D_MODEL // NA_HEADS
NA_KH = 8
NA_KW = 16

N_EXPERTS = 16
EXPERT_FF = 1408
EC_FACTOR = 2

kernel_name = 'hybrid_mla_natten_ecmoe_dit'


def rmsnorm(x, g):
    xf = x.astype(jnp.float32)
    y = xf * lax.rsqrt(jnp.mean(xf * xf, axis=-1, keepdims=True) + EPS)
    return (y * g.astype(jnp.float32)).astype(x.dtype)


def modulate(h, shift, scale):
    return h * (1.0 + scale) + shift


def axial_angles(n_tokens, rot_dim):
    half = rot_dim // 2
    inv = ROPE_THETA ** (-jnp.arange(0, half, 2, dtype=jnp.float32) / half)
    t = jnp.arange(n_tokens)
    row = (t // GRID_W).astype(jnp.float32)
    col = (t % GRID_W).astype(jnp.float32)
    return row[:, None] * inv[None, :], col[:, None] * inv[None, :]


def rotate(x, ang):
    m = ang.shape[-1]
    cos = jnp.cos(ang)[:, None, :].astype(x.dtype)
    sin = jnp.sin(ang)[:, None, :].astype(x.dtype)
    x1, x2 = x[..., :m], x[..., m:]
    return jnp.concatenate([x1 * cos - x2 * sin, x2 * cos + x1 * sin], axis=-1)


def apply_axial_rope(x, ang_r, ang_c):
    half = x.shape[-1] // 2
    return jnp.concatenate([rotate(x[..., :half], ang_r), rotate(x[..., half:], ang_c)], axis=-1)


def softmax_attend(q, k, v, scale):
    s = jnp.einsum('bqhd,bkhd->bhqk', q, k).astype(jnp.float32) * scale
    p = jax.nn.softmax(s, axis=-1).astype(v.dtype)
    return jnp.einsum('bhqk,bkhd->bqhd', p, v)


def mla_mixer(h_lat, h_ctx, w_in, q_a_gain, kv_a_gain, w_qb, w_kvb, q_gain, k_gain, w_o,
              ang_r, ang_c, with_ctx):
    def project(h, use_rope):
        B, T, _ = h.shape
        a = h @ w_in
        q_c = a[..., :Q_LORA]
        kv_c = a[..., Q_LORA:Q_LORA + KV_LORA]
        k_pe = a[..., Q_LORA + KV_LORA:]
        q = (rmsnorm(q_c, q_a_gain) @ w_qb).reshape(B, T, MLA_HEADS, QK_HEAD)
        kv = (rmsnorm(kv_c, kv_a_gain) @ w_kvb).reshape(B, T, MLA_HEADS, QK_NOPE + V_DIM)
        k = jnp.concatenate(
            [kv[..., :QK_NOPE], jnp.broadcast_to(k_pe[:, :, None, :], (B, T, MLA_HEADS, QK_ROPE))],
            axis=-1)
        v = kv[..., QK_NOPE:]
        q = rmsnorm(q, q_gain)
        k = rmsnorm(k, k_gain)
        if use_rope:
            q = jnp.concatenate([q[..., :QK_NOPE], apply_axial_rope(q[..., QK_NOPE:], ang_r, ang_c)], -1)
            k = jnp.concatenate([k[..., :QK_NOPE], apply_axial_rope(k[..., QK_NOPE:], ang_r, ang_c)], -1)
        return q, k, v

    B, N, _ = h_lat.shape
    scale = QK_HEAD ** -0.5
    q_l, k_l, v_l = project(h_lat, True)
    q_c, k_c, v_c = project(h_ctx, False)
    k_all = jnp.concatenate([k_c, k_l], axis=1)
    v_all = jnp.concatenate([v_c, v_l], axis=1)
    nb = N // Q_BLOCK
    qb = jnp.moveaxis(q_l.reshape(B, nb, Q_BLOCK, MLA_HEADS, QK_HEAD), 1, 0)
    o = lax.map(lambda qq: softmax_attend(qq, k_all, v_all, scale), qb)
    o_lat = jnp.moveaxis(o, 0, 1).reshape(B, N, MLA_HEADS * V_DIM) @ w_o
    o_ctx = None
    if with_ctx:
        L = h_ctx.shape[1]
        o_ctx = softmax_attend(q_c, k_c, v_c, scale).reshape(B, L, MLA_HEADS * V_DIM) @ w_o
    return o_lat, o_ctx


def na_mixer(h_lat, h_ctx, w_qkv, q_gain, k_gain, rpb, w_o, with_ctx):
    def project(h):
        B, T, _ = h.shape
        qkv = (h @ w_qkv).reshape(B, T, 3, NA_HEADS, NA_HEAD_DIM)
        return rmsnorm(qkv[:, :, 0], q_gain), rmsnorm(qkv[:, :, 1], k_gain), qkv[:, :, 2]

    B, N, _ = h_lat.shape
    rows = N // GRID_W
    kh = min(NA_KH, rows)
    scale = NA_HEAD_DIM ** -0.5
    q_l, k_l, v_l = project(h_lat)
    q_c, k_c, v_c = project(h_ctx)
    qg = q_l.reshape(B, rows, GRID_W, NA_HEADS, NA_HEAD_DIM)
    kg = k_l.reshape(B, rows, GRID_W, NA_HEADS, NA_HEAD_DIM)
    vg = v_l.reshape(B, rows, GRID_W, NA_HEADS, NA_HEAD_DIM)

    col = jnp.arange(GRID_W)
    col_start = jnp.clip(col - NA_KW // 2, 0, GRID_W - NA_KW)
    col_mask = (col[None, :] >= col_start[:, None]) & (col[None, :] < col_start[:, None] + NA_KW)
    dc_idx = jnp.clip(col[None, :] - col[:, None], -(NA_KW - 1), NA_KW - 1) + NA_KW - 1
    row_start = jnp.clip(jnp.arange(rows) - kh // 2, 0, rows - kh)

    def row_fn(r):
        r0 = row_start[r]
        k_win = lax.dynamic_slice_in_dim(kg, r0, kh, axis=1)
        v_win = lax.dynamic_slice_in_dim(vg, r0, kh, axis=1)
        q_r = lax.dynamic_index_in_dim(qg, r, axis=1, keepdims=False)
        dr = r0 + jnp.arange(kh) - r
        bias = rpb[:, dr + NA_KH - 1][:, :, dc_idx].transpose(0, 2, 1, 3)
        s_win = (jnp.einsum('bqhd,bjkhd->bhqjk', q_r, k_win).astype(jnp.float32) * scale
                 + bias[None].astype(jnp.float32))
        s_win = jnp.where(col_mask[None, None, :, None, :], s_win, -jnp.inf)
        s_ctx = jnp.einsum('bqhd,bkhd->bhqk', q_r, k_c).astype(jnp.float32) * scale
        s = jnp.concatenate([s_win.reshape(B, NA_HEADS, GRID_W, kh * GRID_W), s_ctx], axis=-1)
        p = jax.nn.softmax(s, axis=-1).astype(v_l.dtype)
        p_win = p[..., :kh * GRID_W].reshape(B, NA_HEADS, GRID_W, kh, GRID_W)
        p_ctx = p[..., kh * GRID_W:]
        return (jnp.einsum('bhqjk,bjkhd->bqhd', p_win, v_win)
                + jnp.einsum('bhqk,bkhd->bqhd', p_ctx, v_c))

    o = lax.map(row_fn, jnp.arange(rows))
    o_lat = jnp.moveaxis(o, 0, 1).reshape(B, N, D_MODEL) @ w_o
    o_ctx = None
    if with_ctx:
        L = h_ctx.shape[1]
        o_ctx = softmax_attend(q_c, k_c, v_c, scale).reshape(B, L, D_MODEL) @ w_o
    return o_lat, o_ctx


def ec_moe(h, router_w, w_gate, w_up, w_down):
    B, T, D = h.shape
    cap = EC_FACTOR * T // N_EXPERTS
    aff = jax.nn.softmax((h @ router_w).astype(jnp.float32), axis=-1)
    g, idx = lax.top_k(jnp.swapaxes(aff, 1, 2), cap)
    xs = jax.vmap(lambda hb, ib: hb[ib])(h, idx)
    hid = (jax.nn.silu(jnp.einsum('becd,edf->becf', xs, w_gate))
           * jnp.einsum('becd,edf->becf', xs, w_up))
    y = jnp.einsum('becf,efd->becd', hid, w_down) * g[..., None].astype(h.dtype)
    return jax.vmap(
        lambda yb, ib: jnp.zeros((T, D), yb.dtype).at[ib.reshape(-1)].add(yb.reshape(-1, D)))(y, idx)


def setup_inputs(seed: int = 0) -> dict:
    key = jax.random.key(seed)
    ks = jax.random.split(key, 28)
    f32 = jnp.float32
    n_mla = (DEPTH + 1) // 2
    n_na = DEPTH // 2

    def nrm(k, shape, scale):
        return jax.random.normal(k, shape, f32) * scale

    def gain(k, shape):
        return 1.0 + 0.05 * jax.random.normal(k, shape, f32)

    return {
        'x': nrm(ks[0], (BATCH, SEQ, D_MODEL), 1.0),
        'c': nrm(ks[1], (BATCH, D_MODEL), 1.0),
        'ctx': nrm(ks[2], (BATCH, CTX_LEN, D_MODEL), 1.0),
        'c_ctx': nrm(ks[3], (D_MODEL,), 1.0),
        'ada_w': nrm(ks[4], (DEPTH, D_MODEL, 6 * D_MODEL), 0.5 * D_MODEL ** -0.5),
        'ada_b': nrm(ks[5], (DEPTH, 6 * D_MODEL), 0.02),
        'norm_mix': gain(ks[6], (DEPTH, D_MODEL)),
        'norm_ffn': gain(ks[7], (DEPTH, D_MODEL)),
        'mla_w_in': nrm(ks[8], (n_mla, D_MODEL, Q_LORA + KV_LORA + QK_ROPE), D_MODEL ** -0.5),
        'mla_q_a_gain': gain(ks[9], (n_mla, Q_LORA)),
        'mla_kv_a_gain': gain(ks[10], (n_mla, KV_LORA)),
        'mla_w_qb': nrm(ks[11], (n_mla, Q_LORA, MLA_HEADS * QK_HEAD), Q_LORA ** -0.5),
        'mla_w_kvb': nrm(ks[12], (n_mla, KV_LORA, MLA_HEADS * (QK_NOPE + V_DIM)), KV_LORA ** -0.5),
        'mla_q_gain': gain(ks[13], (n_mla, QK_HEAD)),
        'mla_k_gain': gain(ks[14], (n_mla, QK_HEAD)),
        'mla_w_o': nrm(ks[15], (n_mla, MLA_HEADS * V_DIM, D_MODEL), (MLA_HEADS * V_DIM) ** -0.5),
        'na_w_qkv': nrm(ks[16], (n_na, D_MODEL, 3 * D_MODEL), D_MODEL ** -0.5),
        'na_q_gain': gain(ks[17], (n_na, NA_HEAD_DIM)),
        'na_k_gain': gain(ks[18], (n_na, NA_HEAD_DIM)),
        'na_rpb': nrm(ks[19], (n_na, NA_HEADS, 2 * NA_KH - 1, 2 * NA_KW - 1), 0.1),
        'na_w_o': nrm(ks[20], (n_na, D_MODEL, D_MODEL), D_MODEL ** -0.5),
        'router_w': nrm(ks[21], (DEPTH, D_MODEL, N_EXPERTS), D_MODEL ** -0.5),
        'moe_w_gate': nrm(ks[22], (DEPTH, N_EXPERTS, D_MODEL, EXPERT_FF), D_MODEL ** -0.5),
        'moe_w_up': nrm(ks[23], (DEPTH, N_EXPERTS, D_MODEL, EXPERT_FF), D_MODEL ** -0.5),
        'moe_w_down': nrm(ks[24], (DEPTH, N_EXPERTS, EXPERT_FF, D_MODEL), EXPERT_FF ** -0.5),
    }


def reference(x, c, ctx, c_ctx, ada_w, ada_b, norm_mix, norm_ffn,
              mla_w_in, mla_q_a_gain, mla_kv_a_gain, mla_w_qb, mla_w_kvb, mla_q_gain, mla_k_gain, mla_w_o,
              na_w_qkv, na_q_gain, na_k_gain, na_rpb, na_w_o,
              router_w, moe_w_gate, moe_w_up, moe_w_down):
    B, N, _ = x.shape
    ang_r, ang_c = axial_angles(N, QK_ROPE)
    c_act = jax.nn.silu(c)
    cctx_act = jax.nn.silu(c_ctx)
    x_lat, x_ctx = x, ctx
    for i in range(DEPTH):
        with_ctx = i < DEPTH - 1
        mod_l = (c_act @ ada_w[i] + ada_b[i])[:, None, :]
        mod_c = (cctx_act @ ada_w[i] + ada_b[i])[None, None, :]
        sh_a, sc_a, g_a, sh_f, sc_f, g_f = jnp.split(mod_l, 6, axis=-1)
        csh_a, csc_a, cg_a, csh_f, csc_f, cg_f = jnp.split(mod_c, 6, axis=-1)

        h_lat = modulate(rmsnorm(x_lat, norm_mix[i]), sh_a, sc_a)
        h_ctx = modulate(rmsnorm(x_ctx, norm_mix[i]), csh_a, csc_a)
        j = i // N_MIXERS
        if i % N_MIXERS == 0:
            o_lat, o_ctx = mla_mixer(h_lat, h_ctx, mla_w_in[j], mla_q_a_gain[j], mla_kv_a_gain[j],
                                     mla_w_qb[j], mla_w_kvb[j], mla_q_gain[j], mla_k_gain[j],
                                     mla_w_o[j], ang_r, ang_c, with_ctx)
        else:
            o_lat, o_ctx = na_mixer(h_lat, h_ctx, na_w_qkv[j], na_q_gain[j], na_k_gain[j],
                                    na_rpb[j], na_w_o[j], with_ctx)
        x_lat = x_lat + g_a * o_lat

        f_lat = modulate(rmsnorm(x_lat, norm_ffn[i]), sh_f, sc_f)
        x_lat = x_lat + g_f * ec_moe(f_lat, router_w[i], moe_w_gate[i], moe_w_up[i], moe_w_down[i])

        if with_ctx:
            x_ctx = x_ctx + cg_a * o_ctx
            f_ctx = modulate(rmsnorm(x_ctx, norm_ffn[i]), csh_f, csc_f)
            x_ctx = x_ctx + cg_f * ec_moe(f_ctx, router_w[i], moe_w_gate[i], moe_w_up[i], moe_w_down[i])
    return x_lat
```

```python
import numpy as np
import ml_dtypes
import concourse.bass as bass
import concourse.mybir as mybir
from concourse.bass_utils import run_bass_kernel_spmd

F32 = mybir.dt.float32
BF16 = mybir.dt.bfloat16
I32 = mybir.dt.int32
ALU = mybir.AluOpType
AF = mybir.ActivationFunctionType
AX = mybir.AxisListType

ENGS = ("pe", "dve", "act", "pool", "sp")
NCORES = 8
D = 2048
EPS = 1e-6
NLAT = 16384
NCTX = 256
TPC = NLAT // NCORES


class KB:
    def __init__(self, nc):
        self.nc = nc
        self.q = {e: [] for e in ENGS}
        self.cnt = {e: 0 for e in ENGS}
        self.sem_names = {e: "c_" + e for e in ENGS}
        self.dma_cnt = {}
        self.waited = {e: {} for e in ENGS}
        self.lastw = {}
        self.reads = {}
        self.stack = []
        self.sems = {}
        self.nbank = 0
        self.banks = None

    def sb(self, name, shape, dt):
        return self._enter(self.nc.sbuf_tensor("s_" + name, shape, dt))

    def ps(self, name, shape, dt=F32):
        return self._enter(self.nc.psum_tensor("p_" + name, shape, dt))

    def _enter(self, cm):
        v = cm.__enter__()
        self.stack.append(cm)
        return v

    def alloc_banks(self, n=8):
        self.banks = [self.ps(f"pb{i}", [128, 512], F32) for i in range(n)]

    def bank(self):
        i = self.nbank % len(self.banks)
        self.nbank += 1
        return self.banks[i], f"pb{i}"

    def _deps(self, eng, reads, writes, wadd=()):
        need = {}

        def add(ev):
            if ev is None:
                return
            s, v = ev
            if need.get(s, 0) < v:
                need[s] = v

        for k in reads:
            for ev in self.lastw.get(k, ()):
                add(ev)
        for k in writes:
            for ev in self.lastw.get(k, ()):
                add(ev)
            for ev in self.reads.get(k, ()):
                add(ev)
        for k in wadd:
            for ev in self.reads.get(k, ()):
                add(ev)
        out = []
        for s, v in need.items():
            if eng == "pe" and s == self.sem_names["pe"]:
                continue
            if self.waited[eng].get(s, 0) >= v:
                continue
            self.waited[eng][s] = v
            out.append((s, v))
        return out

    def _commit(self, ev, reads, writes, wadd=()):
        for k in reads:
            self.reads.setdefault(k, []).append(ev)
        for k in writes:
            self.lastw[k] = [ev]
            self.reads[k] = []
        for k in wadd:
            self.lastw.setdefault(k, []).append(ev)

    def op(self, eng, fn, reads=(), writes=()):
        waits = self._deps(eng, reads, writes)
        self.cnt[eng] += 1
        ev = (self.sem_names[eng], self.cnt[eng])
        self.q[eng].append((fn, waits, (ev[0], 1)))
        self._commit(ev, reads, writes)
        return ev

    def dma(self, eng, fn, sem, reads=(), writes=(), wadd=()):
        waits = self._deps(eng, reads, writes, wadd)
        self.dma_cnt[sem] = self.dma_cnt.get(sem, 0) + 16
        ev = ("d_" + sem, self.dma_cnt[sem])
        self.q[eng].append((fn, waits, (ev[0], 16)))
        self._commit(ev, reads, writes, wadd)
        return ev

    def wait_all(self, eng):
        waits = []
        for e in ENGS:
            if self.cnt[e] and self.waited[eng].get(self.sem_names[e], 0) < self.cnt[e]:
                waits.append((self.sem_names[e], self.cnt[e]))
        for s, v in self.dma_cnt.items():
            if self.waited[eng].get("d_" + s, 0) < v:
                waits.append(("d_" + s, v))
        self.q[eng].append((None, waits, None))

    def emit(self):
        nc = self.nc
        names = set()
        for e in ENGS:
            for fn, waits, inc in self.q[e]:
                for s, _ in waits:
                    names.add(s)
                if inc:
                    names.add(inc[0])
        for n in sorted(names):
            self.sems[n] = self._enter(nc.semaphore(n))
        block = self._enter(nc.Block())
        engobj = {"pe": "tensor", "dve": "vector", "act": "scalar", "pool": "gpsimd", "sp": "sync"}

        def mk(e):
            def body(eng):
                for fn, waits, inc in self.q[e]:
                    for s, v in waits:
                        eng.wait_ge(self.sems[s], v)
                    if fn is not None:
                        fn(eng).then_inc(self.sems[inc[0]], inc[1])
            return body

        for e in ENGS:
            if self.q[e]:
                getattr(block, engobj[e])(mk(e))

    def finish(self):
        self.wait_all("sp")
        self.emit()
        while self.stack:
            self.stack.pop().__exit__(None, None, None)


_REGS = {}


def breg(e, val):
    key = (id(e), val)
    if key not in _REGS:
        _REGS[key] = (e, e.to_reg(val))
    return _REGS[key][1]


def _din(nc, name, shape, dt=F32):
    return nc.dram_tensor(name, list(shape), dt, kind="ExternalInput").ap()


def _dout(nc, name, shape, dt=F32):
    return nc.dram_tensor(name, list(shape), dt, kind="ExternalOutput").ap()


def load_consts(kb, nc):
    ones_b = kb.sb("ones_b", [128, 128], BF16)
    ones_f = kb.sb("ones_f", [128, 128], F32)
    kb.op("dve", lambda e: e.memset(ones_b[:], 1.0), writes=["ones_b"])
    kb.op("dve", lambda e: e.memset(ones_f[:], 1.0), writes=["ones_f"])
    kb.eps = kb.sb("eps", [128, 1], F32)
    kb.op("dve", lambda e: e.memset(kb.eps[:], EPS), writes=["eps"])
    return ones_b, ones_f


def rstd_from_psum(kb, ps, pkey, out, okey, W, n, rows=128):
    kb.op("act", lambda e: e.activation(out=out[:rows, :W], in_=ps[:rows, :W], func=AF.Sqrt, scale=1.0 / n, bias=kb.eps[:rows, :]),
          reads=[pkey, "eps"], writes=[okey])
    kb.op("dve", lambda e: e.reciprocal(out=out[:rows, :W], in_=out[:rows, :W]), reads=[okey], writes=[okey])


def build_mod():
    nc = bass.Bass("TRN2", target_bir_lowering=False)
    cc = _din(nc, "cc", [128, 16, 2])
    aw = _din(nc, "aw", [2, 2048, 1536])
    ab = _din(nc, "ab", [2, 1536])
    out = _dout(nc, "out", [2, 2, 1536])
    kb = KB(nc)
    cs = kb.sb("cs", [128, 16, 2], F32)
    sg = kb.sb("sg", [128, 16, 2], F32)
    w = [kb.sb(f"w{i}", [128, 16, 512], F32) for i in range(3)]
    bb = kb.sb("bb", [2, 2, 1536], F32)
    res = kb.sb("res", [2, 2, 1536], F32)
    pst = [kb.ps(f"ps{i}", [2, 512], F32) for i in range(3)]
    kb.dma("sp", lambda e: e.dma_start(out=cs[:], in_=cc), "cs", writes=["cs"])
    for l in range(2):
        for s in range(2):
            kb.dma("sp", lambda e, l=l, s=s: e.dma_start(out=bb[s:s + 1, l, :], in_=ab[l:l + 1, :]), "bb", writes=["bb"])
    kb.op("act", lambda e: e.activation(out=sg[:], in_=cs[:], func=AF.Silu), reads=["cs"], writes=["sg"])
    for l in range(2):
        for nb in range(3):
            kb.dma("sp", lambda e, l=l, nb=nb: e.dma_start(
                out=w[nb][:], in_=aw[l, :, nb * 512:(nb + 1) * 512].rearrange("(kc p) n -> p kc n", p=128)),
                f"w{nb}", writes=[f"w{nb}"])
            for kc in range(16):
                kb.op("pe", lambda e, nb=nb, kc=kc: e.matmul(pst[nb][:], lhsT=sg[:, kc, :], rhs=w[nb][:, kc, :],
                                                             start=(kc == 0), stop=(kc == 15)),
                      reads=["sg", f"w{nb}"], writes=[f"ps{nb}"])
            kb.op("dve", lambda e, l=l, nb=nb: e.tensor_tensor(out=res[:, l, nb * 512:(nb + 1) * 512], in0=pst[nb][:],
                                                               in1=bb[:, l, nb * 512:(nb + 1) * 512], op=ALU.add),
                  reads=[f"ps{nb}", "bb"], writes=["res"])
    kb.dma("sp", lambda e: e.dma_start(out=out.rearrange("l s n -> s l n"), in_=res[:]), "out", reads=["res"])
    kb.finish()
    return nc


def run_mod(I):
    nc = build_mod()
    ccn = np.stack([I["c"][0], I["c_ctx"]], axis=-1).reshape(16, 128, 2).transpose(1, 0, 2).copy()
    in_maps = []
    for k in range(NCORES):
        in_maps.append({"cc": ccn, "aw": np.ascontiguousarray(I["ada_w"][:, :, k * 1536:(k + 1) * 1536]),
                        "ab": np.ascontiguousarray(I["ada_b"][:, k * 1536:(k + 1) * 1536])})
    r = run_bass_kernel_spmd(nc, in_maps, core_ids=list(range(NCORES)))
    return np.concatenate([r.results[k]["out"] for k in range(NCORES)], axis=-1)


def pvec(v):
    return np.ascontiguousarray(np.asarray(v).reshape(-1, 128).T)


def K(name, it):
    return [f"{name}{i}" for i in it]


def norm_mod_block(kb, xb, xkeys, hs, rs, tmp, A, B, s, W, ones_b):
    for kc in range(16):
        kb.op("act", lambda e, kc=kc: e.activation(out=hs[:, kc, :W], in_=xb[:, kc, :W], func=AF.Square),
              reads=[xkeys[kc]], writes=[f"hs{kc}"])
    ps, pk = kb.bank()
    for kc in range(16):
        kb.op("pe", lambda e, kc=kc: e.matmul(ps[:, :W], lhsT=ones_b[:], rhs=hs[:, kc, :W], start=(kc == 0), stop=(kc == 15)),
              reads=[f"hs{kc}", "ones_b"], writes=[pk])
    rstd_from_psum(kb, ps, pk, rs, "rs", W, D)
    for kc in range(16):
        t = tmp[kc % 2]
        kb.op("dve", lambda e, kc=kc, t=t: e.tensor_tensor(out=t[:, :W], in0=xb[:, kc, :W], in1=rs[:, :W], op=ALU.mult),
              reads=[xkeys[kc], "rs"], writes=[f"tmp{kc % 2}"])
        kb.op("act", lambda e, kc=kc, t=t: e.activation(out=hs[:, kc, :W], in_=t[:, :W], func=AF.Identity,
                                                        scale=A[:, kc, s:s + 1], bias=B[:, kc, s:s + 1]),
              reads=[f"tmp{kc % 2}", "AB"], writes=[f"hs{kc}"])


def make_AB(kb, vec, A, B, ig, isc, ish, ncols):
    for j in range(ncols):
        kb.op("dve", lambda e, j=j: e.scalar_tensor_tensor(out=A[:, :, j], in0=vec[:, :, isc[j]], scalar=1.0, in1=vec[:, :, ig],
                                                           op0=ALU.add, op1=ALU.mult), reads=["vec"], writes=["AB"])
        kb.op("dve", lambda e, j=j: e.tensor_copy(out=B[:, :, j], in_=vec[:, :, ish[j]]), reads=["vec"], writes=["AB"])


NT1 = TPC + NCTX
WB1 = 256
BLOCKS1 = [(i * 256, 256, 0) for i in range(8)] + [(2048, 256, 1)]


def build_qkv0():
    nc = bass.Bass("TRN2", target_bir_lowering=False)
    xT = _din(nc, "xT", [D, NT1])
    vecd = _din(nc, "vec", [128, 16, 5])
    gaind = _din(nc, "gains", [128, 16])
    ropeC = _din(nc, "ropeC", [64, NT1])
    ropeS = _din(nc, "ropeS", [64, NT1])
    w_in = _din(nc, "w_in", [D, 1152])
    w_qb = _din(nc, "w_qb", [512, 4096])
    w_kn = _din(nc, "w_kn", [512, 2048])
    w_v = _din(nc, "w_v", [512, 2048])
    qn_o = _dout(nc, "qn", [128, 16, NT1], BF16)
    qr_o = _dout(nc, "qr", [64, 16, NT1], BF16)
    kn_o = _dout(nc, "kn", [128, 16, NT1], BF16)
    kr_o = _dout(nc, "kr", [64, 16, NT1], BF16)
    v_o = _dout(nc, "v", [NT1, 2048], BF16)
    kb = KB(nc)
    kb.alloc_banks(8)
    ones_b, ones_f = load_consts(kb, nc)
    vec = kb.sb("vec", [128, 16, 5], F32)
    gains = kb.sb("gains", [128, 16], F32)
    A = kb.sb("A", [128, 16, 2], F32)
    B = kb.sb("B", [128, 16, 2], F32)
    Cb = kb.sb("Cb", [64, WB1], F32)
    Sb = kb.sb("Sb", [64, WB1], F32)
    Cq = kb.sb("Cq", [64, WB1], F32)
    Sq = kb.sb("Sq", [64, WB1], F32)
    Ck = kb.sb("Ck", [64, WB1], F32)
    Sk = kb.sb("Sk", [64, WB1], F32)
    wi = kb.sb("wi", [128, 16, 1152], BF16)
    wq = kb.sb("wq", [128, 4, 4096], BF16)
    wkn = kb.sb("wkn", [128, 4, 2048], BF16)
    wv = kb.sb("wv", [128, 4, 2048], BF16)
    xb = kb.sb("xb", [128, 16, WB1], F32)
    hs = kb.sb("hs", [128, 16, WB1], BF16)
    rs = kb.sb("rs", [128, WB1], F32)
    tmp = [kb.sb(f"tmp{i}", [128, WB1], F32) for i in range(2)]
    a_sb = kb.sb("a_sb", [128, 10, WB1], F32)
    sq4 = kb.sb("sq4", [128, 4, WB1], BF16)
    qn = kb.sb("qnrm", [128, 4, WB1], BF16)
    kvn = kb.sb("kvnrm", [128, 4, WB1], BF16)
    rq = kb.sb("rq", [128, WB1], F32)
    sqa = [kb.sb(f"sqa{i}", [128, WB1], BF16) for i in range(2)]
    sqb = [kb.sb(f"sqb{i}", [128, WB1], BF16) for i in range(2)]
    rh = [kb.sb(f"rh{i}", [128, WB1], F32) for i in range(2)]
    u = [kb.sb(f"u{i}", [64, WB1], F32) for i in range(2)]
    v2 = [kb.sb(f"v2{i}", [64, WB1], F32) for i in range(2)]
    krb = kb.sb("krb", [64, WB1], F32)
    sqk = kb.sb("sqk", [128, WB1], BF16)
    for t_, k_ in ((sqb[0], "sqb0"), (sqb[1], "sqb1"), (sqk, "sqk")):
        kb.op("dve", lambda e, t_=t_: e.memset(t_[64:128, :], 0.0), writes=[k_])
    stn = [kb.sb(f"stn{i}", [128, 4, WB1], BF16) for i in range(2)]
    str_ = [kb.sb(f"str{i}", [64, 4, WB1], BF16) for i in range(2)]
    vst = [kb.sb(f"vst{i}", [128, 2048], BF16) for i in range(2)]

    kb.dma("sp", lambda e: e.dma_start(out=vec[:], in_=vecd), "vec", writes=["vec"])
    kb.dma("sp", lambda e: e.dma_start(out=gains[:], in_=gaind), "gains", writes=["gains"])
    for kc in range(16):
        kb.dma("pool", lambda e, kc=kc: e.dma_start(out=wi[:, kc, :], in_=w_in[kc * 128:(kc + 1) * 128, :]), "wi", writes=["wi"])
    for kc in range(4):
        kb.dma("pool", lambda e, kc=kc: e.dma_start(out=wq[:, kc, :], in_=w_qb[kc * 128:(kc + 1) * 128, :]), "wq", writes=["wq"])
        kb.dma("pool", lambda e, kc=kc: e.dma_start(out=wkn[:, kc, :], in_=w_kn[kc * 128:(kc + 1) * 128, :]), "wkn", writes=["wkn"])
        kb.dma("pool", lambda e, kc=kc: e.dma_start(out=wv[:, kc, :], in_=w_v[kc * 128:(kc + 1) * 128, :]), "wv", writes=["wv"])
    make_AB(kb, vec, A, B, 0, [1, 3], [2, 4], 2)

    for (c0, W, s) in BLOCKS1:
        kb.dma("sp", lambda e, c0=c0, W=W: e.dma_start(out=xb[:, :, :W], in_=xT[:, c0:c0 + W].rearrange("(kc p) n -> p kc n", p=128)),
               "xb", writes=K("xb", range(16)))
        kb.dma("sp", lambda e, c0=c0, W=W: e.dma_start(out=Cb[:, :W], in_=ropeC[:, c0:c0 + W]), "ropeC", writes=["Cb"])
        kb.dma("sp", lambda e, c0=c0, W=W: e.dma_start(out=Sb[:, :W], in_=ropeS[:, c0:c0 + W]), "ropeS", writes=["Sb"])
        for T, key, col, src, sk in ((Cq, "Cq", 9, Cb, "Cb"), (Sq, "Sq", 10, Sb, "Sb"), (Ck, "Ck", 12, Cb, "Cb"), (Sk, "Sk", 13, Sb, "Sb")):
            kb.op("pool", lambda e, T=T, col=col, src=src: e.tensor_scalar(out=T[:, :W], in0=src[:, :W], scalar1=gains[0:64, col:col + 1], scalar2=None,
                                                                 op0=ALU.mult), reads=["gains", sk], writes=[key])
        norm_mod_block(kb, xb, K("xb", range(16)), hs, rs, tmp, A, B, s, W, ones_b)
        for m in range(10):
            mw = 128 if m < 8 else 64
            mc0 = m * 128 if m < 8 else 1024 + (m - 8) * 64
            ps, pk = kb.bank()
            for kc in range(16):
                kb.op("pe", lambda e, kc=kc, mw=mw, mc0=mc0, ps=ps: e.matmul(ps[:mw, :W], lhsT=wi[:, kc, mc0:mc0 + mw], rhs=hs[:, kc, :W],
                                                                             start=(kc == 0), stop=(kc == 15)),
                      reads=[f"hs{kc}", "wi"], writes=[pk])
            eng = "act" if m % 2 == 0 else "dve"
            if eng == "act":
                kb.op("act", lambda e, m=m, mw=mw, ps=ps: e.activation(out=a_sb[:mw, m, :W], in_=ps[:mw, :W], func=AF.Copy),
                      reads=[pk], writes=[f"a{m}"])
            else:
                kb.op("dve", lambda e, m=m, mw=mw, ps=ps: e.tensor_copy(out=a_sb[:mw, m, :W], in_=ps[:mw, :W]),
                      reads=[pk], writes=[f"a{m}"])
        for (base, dst, dk, gcol) in ((0, qn, "qn", 0), (4, kvn, "kvn", 4)):
            for m in range(4):
                kb.op("act", lambda e, m=m, base=base: e.activation(out=sq4[:, m, :W], in_=a_sb[:, base + m, :W], func=AF.Square),
                      reads=[f"a{base + m}"], writes=[f"sq4{m}"])
            ps, pk = kb.bank()
            for m in range(4):
                kb.op("pe", lambda e, m=m, ps=ps: e.matmul(ps[:, :W], lhsT=ones_b[:], rhs=sq4[:, m, :W], start=(m == 0), stop=(m == 3)),
                      reads=[f"sq4{m}", "ones_b"], writes=[pk])
            rstd_from_psum(kb, ps, pk, rq, "rq", W, 512)
            for m in range(4):
                kb.op("dve", lambda e, m=m, base=base, dst=dst, gcol=gcol: e.scalar_tensor_tensor(
                    out=dst[:, m, :W], in0=a_sb[:, base + m, :W], scalar=gains[:, gcol + m:gcol + m + 1], in1=rq[:, :W],
                    op0=ALU.mult, op1=ALU.mult), reads=[f"a{base + m}", "gains", "rq"], writes=[f"{dk}{m}"])
        kb.op("act", lambda e: e.activation(out=sqk[0:64, :W], in_=a_sb[:64, 8, :W], func=AF.Square), reads=["a8"], writes=["sqk"])
        kb.op("dve", lambda e: e.tensor_tensor(out=krb[:, :W], in0=a_sb[:64, 8, :W], in1=Ck[:, :W], op=ALU.mult),
              reads=["a8", "Ck"], writes=["krb"])
        kb.op("dve", lambda e: e.tensor_tensor(out=u[0][:, :W], in0=a_sb[:64, 9, :W], in1=Sk[:, :W], op=ALU.mult),
              reads=["a9", "Sk"], writes=["u0"])
        kb.op("dve", lambda e: e.tensor_tensor(out=krb[:, :W], in0=krb[:, :W], in1=u[0][:, :W], op=ALU.add),
              reads=["u0", "krb"], writes=["krb"])
        for h in range(16):
            i2 = h % 2
            psn, pkn = kb.bank()
            psr, pkr = kb.bank()
            pss, pks = kb.bank()
            for kc in range(4):
                kb.op("pe", lambda e, kc=kc, h=h, psn=psn: e.matmul(psn[:, :W], lhsT=wq[:, kc, h * 256:h * 256 + 128], rhs=qn[:, kc, :W],
                                                                    start=(kc == 0), stop=(kc == 3)), reads=[f"qn{kc}", "wq"], writes=[pkn])
            for kc in range(4):
                kb.op("pe", lambda e, kc=kc, h=h, psr=psr: e.matmul(psr[:64, :W], lhsT=wq[:, kc, h * 256 + 128:h * 256 + 192], rhs=qn[:, kc, :W],
                                                                    start=(kc == 0), stop=(kc == 3)), reads=[f"qn{kc}", "wq"], writes=[pkr])
            for kc in range(4):
                kb.op("pe", lambda e, kc=kc, h=h, pss=pss: e.matmul(pss[:64, :W], lhsT=wq[:, kc, h * 256 + 192:h * 256 + 256], rhs=qn[:, kc, :W],
                                                                    start=(kc == 0), stop=(kc == 3)), reads=[f"qn{kc}", "wq"], writes=[pks])
            kb.op("act", lambda e, i2=i2, psn=psn: e.activation(out=sqa[i2][:, :W], in_=psn[:, :W], func=AF.Square), reads=[pkn], writes=[f"sqa{i2}"])
            kb.op("act", lambda e, i2=i2, psr=psr: e.activation(out=sqb[i2][0:64, :W], in_=psr[:64, :W], func=AF.Square), reads=[pkr], writes=[f"sqb{i2}"])
            pq, pkq = kb.bank()
            kb.op("pe", lambda e, i2=i2, pq=pq: e.matmul(pq[:, :W], lhsT=ones_b[:], rhs=sqa[i2][:, :W], start=True, stop=False),
                  reads=[f"sqa{i2}", "ones_b"], writes=[pkq])
            kb.op("pe", lambda e, i2=i2, pq=pq: e.matmul(pq[:, :W], lhsT=ones_b[:], rhs=sqb[i2][:, :W], start=False, stop=True),
                  reads=[f"sqb{i2}", "ones_b"], writes=[pkq])
            rstd_from_psum(kb, pq, pkq, rh[i2], f"rh{i2}", W, 192)
            kb.op("dve", lambda e, h=h, i2=i2, psn=psn: e.scalar_tensor_tensor(out=stn[(h // 4) % 2][:, h % 4, :W], in0=psn[:, :W], scalar=gains[:, 8:9], in1=rh[i2][:, :W],
                                                                               op0=ALU.mult, op1=ALU.mult), reads=[pkn, "gains", f"rh{i2}"], writes=[f"stn{(h // 4) % 2}"])
            kb.op("dve", lambda e, i2=i2, psr=psr: e.tensor_tensor(out=u[i2][:, :W], in0=psr[:64, :W], in1=Cq[:, :W], op=ALU.mult),
                  reads=[pkr, "Cq"], writes=[f"u{i2}"])
            kb.op("dve", lambda e, i2=i2, pss=pss: e.tensor_tensor(out=v2[i2][:, :W], in0=pss[:64, :W], in1=Sq[:, :W], op=ALU.mult),
                  reads=[pks, "Sq"], writes=[f"v2{i2}"])
            kb.op("pool", lambda e, i2=i2: e.tensor_tensor(out=u[i2][:, :W], in0=u[i2][:, :W], in1=v2[i2][:, :W], op=ALU.add),
                  reads=[f"u{i2}", f"v2{i2}"], writes=[f"u{i2}"])
            kb.op("pool", lambda e, h=h, i2=i2: e.tensor_tensor(out=str_[(h // 4) % 2][:, h % 4, :W], in0=u[i2][:, :W], in1=rh[i2][:64, :W], op=ALU.mult),
                  reads=[f"u{i2}", f"rh{i2}"], writes=[f"str{(h // 4) % 2}"])
            if h % 4 == 3:
                g4 = h // 4
                kb.dma("sp", lambda e, c0=c0, W=W, g4=g4: e.dma_start(out=qn_o[:, g4 * 4:g4 * 4 + 4, c0:c0 + W], in_=stn[g4 % 2][:, :, :W]), f"stn{g4 % 2}", reads=[f"stn{g4 % 2}"])
                kb.dma("sp", lambda e, c0=c0, W=W, g4=g4: e.dma_start(out=qr_o[:, g4 * 4:g4 * 4 + 4, c0:c0 + W], in_=str_[g4 % 2][:, :, :W]), f"str{g4 % 2}", reads=[f"str{g4 % 2}"])
        for h in range(16):
            i2 = h % 2
            psn, pkn = kb.bank()
            for kc in range(4):
                kb.op("pe", lambda e, kc=kc, h=h, psn=psn: e.matmul(psn[:, :W], lhsT=wkn[:, kc, h * 128:(h + 1) * 128], rhs=kvn[:, kc, :W],
                                                                    start=(kc == 0), stop=(kc == 3)), reads=[f"kvn{kc}", "wkn"], writes=[pkn])
            kb.op("act", lambda e, i2=i2, psn=psn: e.activation(out=sqa[i2][:, :W], in_=psn[:, :W], func=AF.Square), reads=[pkn], writes=[f"sqa{i2}"])
            pq, pkq = kb.bank()
            kb.op("pe", lambda e, i2=i2, pq=pq: e.matmul(pq[:, :W], lhsT=ones_b[:], rhs=sqa[i2][:, :W], start=True, stop=False),
                  reads=[f"sqa{i2}", "ones_b"], writes=[pkq])
            kb.op("pe", lambda e, pq=pq: e.matmul(pq[:, :W], lhsT=ones_b[:], rhs=sqk[:, :W], start=False, stop=True),
                  reads=["sqk", "ones_b"], writes=[pkq])
            rstd_from_psum(kb, pq, pkq, rh[i2], f"rh{i2}", W, 192)
            kb.op("dve", lambda e, h=h, i2=i2, psn=psn: e.scalar_tensor_tensor(out=stn[(h // 4) % 2][:, h % 4, :W], in0=psn[:, :W], scalar=gains[:, 11:12], in1=rh[i2][:, :W],
                                                                               op0=ALU.mult, op1=ALU.mult), reads=[pkn, "gains", f"rh{i2}"], writes=[f"stn{(h // 4) % 2}"])
            kb.op("pool", lambda e, h=h, i2=i2: e.tensor_tensor(out=str_[(h // 4) % 2][:, h % 4, :W], in0=krb[:, :W], in1=rh[i2][:64, :W], op=ALU.mult),
                  reads=["krb", f"rh{i2}"], writes=[f"str{(h // 4) % 2}"])
            if h % 4 == 3:
                g4 = h // 4
                kb.dma("sp", lambda e, c0=c0, W=W, g4=g4: e.dma_start(out=kn_o[:, g4 * 4:g4 * 4 + 4, c0:c0 + W], in_=stn[g4 % 2][:, :, :W]), f"stn{g4 % 2}", reads=[f"stn{g4 % 2}"])
                kb.dma("sp", lambda e, c0=c0, W=W, g4=g4: e.dma_start(out=kr_o[:, g4 * 4:g4 * 4 + 4, c0:c0 + W], in_=str_[g4 % 2][:, :, :W]), f"str{g4 % 2}", reads=[f"str{g4 % 2}"])
        for tt in range(W // 128):
            vb = vst[tt % 2]
            for nb in range(4):
                ps, pk = kb.bank()
                for kc in range(4):
                    kb.op("pe", lambda e, kc=kc, nb=nb, tt=tt, ps=ps: e.matmul(ps[:, :], lhsT=kvn[:, kc, tt * 128:(tt + 1) * 128],
                                                                               rhs=wv[:, kc, nb * 512:(nb + 1) * 512], start=(kc == 0), stop=(kc == 3)),
                          reads=[f"kvn{kc}", "wv"], writes=[pk])
                if nb % 2 == 0:
                    kb.op("act", lambda e, nb=nb, vb=vb, ps=ps: e.activation(out=vb[:, nb * 512:(nb + 1) * 512], in_=ps[:, :], func=AF.Copy),
                          reads=[pk], writes=[f"vst{tt % 2}"])
                else:
                    kb.op("dve", lambda e, nb=nb, vb=vb, ps=ps: e.tensor_copy(out=vb[:, nb * 512:(nb + 1) * 512], in_=ps[:, :]),
                          reads=[pk], writes=[f"vst{tt % 2}"])
            kb.dma("sp", lambda e, tt=tt, vb=vb, c0=c0: e.dma_start(out=v_o[c0 + tt * 128:c0 + (tt + 1) * 128, :], in_=vb[:]), f"vo{tt % 2}",
                   reads=[f"vst{tt % 2}"])
    kb.finish()
    return nc


def rope_tables():
    half = 32
    inv = (10000.0 ** (-np.arange(0, half, 2, dtype=np.float32) / half)).astype(np.float32)
    t = np.arange(NLAT)
    row = (t // 64).astype(np.float32)
    col = (t % 64).astype(np.float32)
    ang = [row[:, None] * inv[None, :], col[:, None] * inv[None, :]]
    C = np.zeros((64, NLAT), np.float32)
    S = np.zeros((64, NLAT), np.float32)
    for p in range(64):
        a = ang[p // 32][:, p % 16]
        C[p] = np.cos(a)
        S[p] = -np.sin(a) if (p % 32) < 16 else np.sin(a)
    return C, S


ROPE_SW = np.array([p + 16 if (p % 32) < 16 else p - 16 for p in range(64)])


def prep_qkv0(I, mod):
    C, S = rope_tables()
    w_in = I["mla_w_in"][0]
    w_in_ext = np.concatenate([w_in, w_in[:, 1024 + ROPE_SW]], axis=1)
    wqb = I["mla_w_qb"][0].reshape(512, 16, 192)
    w_qb_ext = np.concatenate([wqb, wqb[:, :, 128 + ROPE_SW]], axis=2).reshape(512, 4096)
    wkvb = I["mla_w_kvb"][0].reshape(512, 16, 256)
    w_kn = np.ascontiguousarray(wkvb[:, :, :128]).reshape(512, 2048)
    w_v = np.ascontiguousarray(wkvb[:, :, 128:]).reshape(512, 2048)
    ml, mc = mod[0, 0], mod[0, 1]
    vec = np.stack([pvec(I["norm_mix"][0]), pvec(ml[D:2 * D]), pvec(ml[0:D]), pvec(mc[D:2 * D]), pvec(mc[0:D])], axis=-1)
    gains = np.zeros((128, 16), np.float32)
    gains[:, 0:4] = I["mla_q_a_gain"][0].reshape(4, 128).T
    gains[:, 4:8] = I["mla_kv_a_gain"][0].reshape(4, 128).T
    gq, gk = I["mla_q_gain"][0], I["mla_k_gain"][0]
    gains[:, 8] = gq[:128]
    gains[:64, 9] = gq[128:]
    gains[:64, 10] = gq[128 + ROPE_SW]
    gains[:, 11] = gk[:128]
    gains[:64, 12] = gk[128:]
    gains[:64, 13] = gk[128 + ROPE_SW]
    ctxT = np.ascontiguousarray(I["ctx"][0].T)
    Cc = np.ones((64, NCTX), np.float32)
    Sc = np.zeros((64, NCTX), np.float32)
    maps = []
    for k in range(NCORES):
        sl = slice(k * TPC, (k + 1) * TPC)
        maps.append({
            "xT": np.concatenate([I["x"][0, sl].T, ctxT], axis=1),
            "vec": vec.astype(np.float32), "gains": gains,
            "ropeC": np.concatenate([C[:, sl], Cc], axis=1), "ropeS": np.concatenate([S[:, sl], Sc], axis=1),
            "w_in": w_in_ext, "w_qb": w_qb_ext, "w_kn": w_kn, "w_v": w_v,
        })
    return maps


NALL = NCTX + NLAT
NKT = NALL // 128
HPC = 2
SCALE0 = 192 ** -0.5


def attn_alloc(kb, dqk_r=64):
    T = {}
    T["st"] = [kb.ps(f"st{i}", [128, 512], F32) for i in range(4)]
    T["oacc"] = [kb.ps(f"oacc{i}", [128, 512], F32) for i in range(2)]
    T["lps"] = [kb.ps(f"lps{i}", [128, 512], F32) for i in range(2)]
    T["qnb"] = [kb.sb(f"qnb{i}", [128, 1024], BF16) for i in range(2)]
    T["qrb"] = [kb.sb(f"qrb{i}", [128, 1024], BF16) for i in range(2)] if dqk_r else None
    if dqk_r:
        for i in range(2):
            kb.op("dve", lambda e, i=i: e.memset(T["qrb"][i][64:128, :], 0.0), writes=[f"qrb{i}"])
    T["pT"] = [kb.sb(f"pT{i}", [128, 512], BF16) for i in range(4)]
    T["sacc"] = [kb.sb(f"sacc{i}", [128, 512], F32) for i in range(2)]
    T["rcp"] = [kb.sb(f"rcp{i}", [128, 512], F32) for i in range(2)]
    T["ost"] = [kb.sb(f"ost{i}", [128, 512], BF16) for i in range(2)]
    return T


def attn_core(kb, T, ones_f, Kn, Kr, V, qn_d, qr_d, o_d, h, groups, scale, dqk_r=64):
    st, oacc, lps, qnb, qrb, pT, sacc, rcp, ost = (T[n] for n in ("st", "oacc", "lps", "qnb", "qrb", "pT", "sacc", "rcp", "ost"))
    nst = 0
    for gi, (q0, widths, nkt) in enumerate(groups):
        gb = gi % 2
        GW = sum(widths)
        kb.dma("sp", lambda e, gb=gb, q0=q0, GW=GW: e.dma_start(out=qnb[gb][:, :GW], in_=qn_d[:, h, q0:q0 + GW]), f"qnb{gb}", writes=[f"qnb{gb}"])
        if dqk_r:
            kb.dma("sp", lambda e, gb=gb, q0=q0, GW=GW: e.dma_start(out=qrb[gb][0:64, :GW], in_=qr_d[:, h, q0:q0 + GW]), f"qrb{gb}", reads=[f"qrb{gb}z"], writes=[f"qrb{gb}"])
        offs = [sum(widths[:i]) for i in range(len(widths))]
        def qk(kt):
            nonlocal nst
            cur = []
            for qb, W in enumerate(widths):
                si = nst % 4
                nst += 1
                o = offs[qb]
                kb.op("pe", lambda e, si=si, kt=kt, gb=gb, o=o, W=W: e.matmul(st[si][:, :W], lhsT=Kn[:, kt * 128:(kt + 1) * 128], rhs=qnb[gb][:, o:o + W],
                                                                             start=True, stop=(not dqk_r)), reads=["Kn", f"qnb{gb}"], writes=[f"st{si}"])
                if dqk_r:
                    kb.op("pe", lambda e, si=si, kt=kt, gb=gb, o=o, W=W: e.matmul(st[si][:, :W], lhsT=Kr[:, kt * 128:(kt + 1) * 128], rhs=qrb[gb][:, o:o + W],
                                                                                 start=False, stop=True), reads=["Kr", f"qrb{gb}"], writes=[f"st{si}"])
                cur.append(si)
            return cur

        nxt = qk(0)
        for kt in range(nkt):
            cur = nxt
            if kt + 1 < nkt:
                nxt = qk(kt + 1)
            for qb, W in enumerate(widths):
                si = cur[qb]
                kb.op("act", lambda e, si=si, W=W: e.activation(out=pT[si][:, :W], in_=st[si][:, :W], func=AF.Exp, scale=scale),
                      reads=[f"st{si}"], writes=[f"pT{si}"])
                kb.op("pe", lambda e, si=si, kt=kt, qb=qb, W=W, nkt=nkt: e.matmul(oacc[qb][:, :W], lhsT=V[:, kt, :], rhs=pT[si][:, :W],
                                                                                 start=(kt == 0), stop=(kt == nkt - 1)), reads=["V", f"pT{si}"], writes=[f"oacc{qb}"])
                ae = "dve"
                if kt == 0:
                    kb.op(ae, lambda e, si=si, qb=qb, W=W: e.tensor_copy(out=sacc[qb][:, :W], in_=pT[si][:, :W]), reads=[f"pT{si}"], writes=[f"sacc{qb}"])
                else:
                    kb.op(ae, lambda e, si=si, qb=qb, W=W: e.tensor_tensor(out=sacc[qb][:, :W], in0=sacc[qb][:, :W], in1=pT[si][:, :W], op=ALU.add),
                          reads=[f"pT{si}", f"sacc{qb}"], writes=[f"sacc{qb}"])
        for qb, W in enumerate(widths):
            kb.op("pe", lambda e, qb=qb, W=W: e.matmul(lps[qb][:, :W], lhsT=ones_f[:], rhs=sacc[qb][:, :W], start=True, stop=True),
                  reads=["ones_f", f"sacc{qb}"], writes=[f"lps{qb}"])
            kb.op("dve", lambda e, qb=qb, W=W: e.reciprocal(out=rcp[qb][:, :W], in_=lps[qb][:, :W]), reads=[f"lps{qb}"], writes=[f"rcp{qb}"])
            kb.op("dve", lambda e, qb=qb, W=W: e.tensor_tensor(out=ost[qb][:, :W], in0=oacc[qb][:, :W], in1=rcp[qb][:, :W], op=ALU.mult),
                  reads=[f"oacc{qb}", f"rcp{qb}"], writes=[f"ost{qb}"])
            o = q0 + offs[qb]
            kb.dma("sp", lambda e, qb=qb, W=W, o=o: e.dma_start(out=o_d[:, h, o:o + W], in_=ost[qb][:, :W]), f"ost{qb}", reads=[f"ost{qb}"])


def build_attn0():
    nc = bass.Bass("TRN2", target_bir_lowering=False)
    qn_d = _din(nc, "qn", [128, HPC, NALL], BF16)
    qr_d = _din(nc, "qr", [64, HPC, NALL], BF16)
    kn_d = _din(nc, "kn", [128, HPC, NALL], BF16)
    kr_d = _din(nc, "kr", [64, HPC, NALL], BF16)
    v_d = _din(nc, "v", [128, HPC, NKT, 128], BF16)
    o_d = _dout(nc, "o", [128, HPC, NALL], BF16)
    kb = KB(nc)
    ones_b, ones_f = load_consts(kb, nc)
    Kn = kb.sb("Kn", [128, NALL], BF16)
    Kr = kb.sb("Kr", [128, NALL], BF16)
    kb.op("dve", lambda e: e.memset(Kr[64:128, :], 0.0), writes=["Kr"])
    V = kb.sb("V", [128, NKT, 128], BF16)
    groups = [(0, [256], 2)] + [(NCTX + g * 1024, [512, 512], NKT) for g in range(NLAT // 1024)]
    T = attn_alloc(kb)
    for h in range(HPC):
        for c in range(4):
            c0, c1 = c * (NALL // 4), (c + 1) * (NALL // 4)
            kb.dma("sp", lambda e, h=h, c0=c0, c1=c1: e.dma_start(out=Kn[:, c0:c1], in_=kn_d[:, h, c0:c1]), "Kn", writes=["Kn"])
            kb.dma("sp", lambda e, h=h, c0=c0, c1=c1: e.dma_start(out=Kr[0:64, c0:c1], in_=kr_d[:, h, c0:c1]), "Kr", writes=["Kr"])
        for c in range(2):
            c0, c1 = c * (NKT // 2), (c + 1) * (NKT // 2)
            kb.dma("sp", lambda e, h=h, c0=c0, c1=c1: e.dma_start(out=V[:, c0:c1, :], in_=v_d[:, h, c0:c1, :]), "V", writes=["V"])
        attn_core(kb, T, ones_f, Kn, Kr, V, qn_d, qr_d, o_d, h, groups, SCALE0)
    kb.finish()
    return nc


def prep_attn0(res1):
    def allcols(name):
        ctxp = np.asarray(res1[0][name])[:, :, TPC:]
        return np.concatenate([ctxp] + [np.asarray(res1[k][name])[:, :, :TPC] for k in range(NCORES)], axis=2)
    qn, qr, kn, kr = allcols("qn"), allcols("qr"), allcols("kn"), allcols("kr")
    v = np.concatenate([np.asarray(res1[0]["v"])[TPC:]] + [np.asarray(res1[k]["v"])[:TPC] for k in range(NCORES)], axis=0)
    v = v.reshape(NKT, 128, 16, 128).transpose(1, 2, 0, 3)
    maps = []
    for k in range(NCORES):
        hs = slice(k * HPC, (k + 1) * HPC)
        maps.append({"qn": np.ascontiguousarray(qn[:, hs]), "qr": np.ascontiguousarray(qr[:, hs]),
                     "kn": np.ascontiguousarray(kn[:, hs]), "kr": np.ascontiguousarray(kr[:, hs]),
                     "v": np.ascontiguousarray(v[:, hs])})
    return maps


def build_post(with_ctx):
    ncols = NT1 if with_ctx else TPC
    blocks = [(i * 256, 256, 0) for i in range(8)] + ([(2048, 256, 1)] if with_ctx else [])
    nc = bass.Bass("TRN2", target_bir_lowering=False)
    xT = _din(nc, "xT", [D, ncols])
    oT = _din(nc, "oT", [128, 16, ncols], BF16)
    vecd = _din(nc, "vec", [128, 16, 7])
    w_o = _din(nc, "w_o", [D, D])
    rwd = _din(nc, "rw", [128, 16, 16])
    x1_o = _dout(nc, "x1T", [D, ncols])
    f_o = _dout(nc, "fT", [D, ncols], BF16)
    aff_o = _dout(nc, "aff", [ncols, 16])
    kb = KB(nc)
    kb.alloc_banks(8)
    ones_b, ones_f = load_consts(kb, nc)
    vec = kb.sb("vec", [128, 16, 7], F32)
    A = kb.sb("A", [128, 16, 2], F32)
    B = kb.sb("B", [128, 16, 2], F32)
    wo = kb.sb("wo", [128, 16, D], BF16)
    rw = kb.sb("rw", [128, 16, 16], F32)
    xb = kb.sb("xb", [128, 16, 256], F32)
    ob = kb.sb("ob", [128, 16, 256], BF16)
    hs = kb.sb("hs", [128, 16, 256], BF16)
    f32b = kb.sb("f32b", [128, 16, 256], F32)
    rs = kb.sb("rs", [128, 256], F32)
    tmp = [kb.sb(f"tmp{i}", [128, 256], F32) for i in range(2)]
    mx = kb.sb("mx", [128, 1], F32)
    se = kb.sb("se", [128, 1], F32)
    ee = kb.sb("ee", [128, 16], F32)
    affs = [kb.sb(f"affs{i}", [128, 16], F32) for i in range(2)]
    kb.dma("sp", lambda e: e.dma_start(out=vec[:], in_=vecd), "vec", writes=["vec"])
    kb.dma("sp", lambda e: e.dma_start(out=rw[:], in_=rwd), "rw", writes=["rw"])
    for h in range(16):
        kb.dma("pool", lambda e, h=h: e.dma_start(out=wo[:, h, :], in_=w_o[h * 128:(h + 1) * 128, :]), "wo", writes=["wo"])
    make_AB(kb, vec, A, B, 2, [3, 5], [4, 6], 2)
    def do_block(c0, W, s):
        kb.dma("sp", lambda e, c0=c0, W=W: e.dma_start(out=xb[:, :, :W], in_=xT[:, c0:c0 + W].rearrange("(kc p) n -> p kc n", p=128)),
               "xb", writes=K("xb", range(16)))
        kb.dma("sp", lambda e, c0=c0, W=W: e.dma_start(out=ob[:, :, :W], in_=oT[:, :, c0:c0 + W]), "ob", writes=["ob"])
        for fc in range(16):
            ps, pk = kb.bank()
            for h in range(16):
                kb.op("pe", lambda e, h=h, fc=fc, ps=ps: e.matmul(ps[:, :W], lhsT=wo[:, h, fc * 128:(fc + 1) * 128], rhs=ob[:, h, :W],
                                                                  start=(h == 0), stop=(h == 15)), reads=["wo", "ob"], writes=[pk])
            kb.op("dve", lambda e, fc=fc, ps=ps: e.scalar_tensor_tensor(out=xb[:, fc, :W], in0=ps[:, :W], scalar=vec[:, fc, s:s + 1], in1=xb[:, fc, :W],
                                                                        op0=ALU.mult, op1=ALU.add), reads=[pk, "vec", f"xb{fc}"], writes=[f"xb{fc}"])
        kb.dma("sp", lambda e, c0=c0, W=W: e.dma_start(out=x1_o[:, c0:c0 + W].rearrange("(kc p) n -> p kc n", p=128), in_=xb[:, :, :W]),
               "x1o", reads=K("xb", range(16)))
        for kc in range(16):
            kb.op("act", lambda e, kc=kc: e.activation(out=hs[:, kc, :W], in_=xb[:, kc, :W], func=AF.Square), reads=[f"xb{kc}"], writes=[f"hs{kc}"])
        ps, pk = kb.bank()
        for kc in range(16):
            kb.op("pe", lambda e, kc=kc, ps=ps: e.matmul(ps[:, :W], lhsT=ones_b[:], rhs=hs[:, kc, :W], start=(kc == 0), stop=(kc == 15)),
                  reads=[f"hs{kc}", "ones_b"], writes=[pk])
        rstd_from_psum(kb, ps, pk, rs, "rs", W, D)
        for kc in range(16):
            t = tmp[kc % 2]
            kb.op("dve", lambda e, kc=kc, t=t: e.tensor_tensor(out=t[:, :W], in0=xb[:, kc, :W], in1=rs[:, :W], op=ALU.mult),
                  reads=[f"xb{kc}", "rs"], writes=[f"tmp{kc % 2}"])
            kb.op("act", lambda e, kc=kc, t=t: e.activation(out=f32b[:, kc, :W], in_=t[:, :W], func=AF.Identity,
                                                            scale=A[:, kc, s:s + 1], bias=B[:, kc, s:s + 1]),
                  reads=[f"tmp{kc % 2}", "AB"], writes=[f"f32b{kc}"])
            kb.op("pool", lambda e, kc=kc: e.tensor_copy(out=hs[:, kc, :W], in_=f32b[:, kc, :W]), reads=[f"f32b{kc}"], writes=[f"hs{kc}"])
        kb.dma("sp", lambda e, c0=c0, W=W: e.dma_start(out=f_o[:, c0:c0 + W].rearrange("(kc p) n -> p kc n", p=128), in_=hs[:, :, :W]),
               "fo", reads=K("hs", range(16)))
        for tt in range(W // 128):
            ps, pk = kb.bank()
            for kc in range(16):
                kb.op("pe", lambda e, kc=kc, tt=tt, ps=ps: e.matmul(ps[:, :16], lhsT=f32b[:, kc, tt * 128:(tt + 1) * 128], rhs=rw[:, kc, :],
                                                                    start=(kc == 0), stop=(kc == 15)), reads=[f"f32b{kc}", "rw"], writes=[pk])
            af = affs[tt % 2]
            kb.op("dve", lambda e, ps=ps: e.reduce_max(out=mx[:], in_=ps[:, :16], axis=AX.X), reads=[pk], writes=["mx"])
            kb.op("dve", lambda e: e.tensor_scalar(out=mx[:], in0=mx[:], scalar1=-1.0, scalar2=None, op0=ALU.mult), reads=["mx"], writes=["mx"])
            kb.op("dve", lambda e: e.memset(se[:], 0.0), writes=["se"])
            kb.op("act", lambda e, ps=ps: e.activation(out=ee[:], in_=ps[:, :16], func=AF.Exp, bias=mx[:, 0:1], scale=1.0, accum_out=se[:]),
                  reads=[pk, "mx", "se"], writes=["ee", "se"])
            kb.op("dve", lambda e: e.reciprocal(out=se[:], in_=se[:]), reads=["se"], writes=["se"])
            kb.op("dve", lambda e, af=af: e.tensor_scalar(out=af[:], in0=ee[:], scalar1=se[:, 0:1], scalar2=None, op0=ALU.mult),
                  reads=["ee", "se"], writes=[f"affs{tt % 2}"])
            kb.dma("sp", lambda e, af=af, c0=c0, tt=tt: e.dma_start(out=aff_o[c0 + tt * 128:c0 + (tt + 1) * 128, :], in_=af[:]), f"affo{tt % 2}",
                   reads=[f"affs{tt % 2}"])
    for blk in blocks:
        do_block(*blk)
    kb.finish()
    return nc


def prep_post0(I, mod, res2, xT_maps):
    o_all = np.concatenate([np.asarray(res2[k]["o"]) for k in range(NCORES)], axis=1)
    ml, mc = mod[0, 0], mod[0, 1]
    vec = np.stack([pvec(ml[2 * D:3 * D]), pvec(mc[2 * D:3 * D]), pvec(I["norm_ffn"][0]),
                    pvec(ml[4 * D:5 * D]), pvec(ml[3 * D:4 * D]), pvec(mc[4 * D:5 * D]), pvec(mc[3 * D:4 * D])], axis=-1).astype(np.float32)
    rw = np.ascontiguousarray(I["router_w"][0].reshape(16, 128, 16).transpose(1, 0, 2))
    maps = []
    for k in range(NCORES):
        oT = np.concatenate([o_all[:, :, NCTX + k * TPC:NCTX + (k + 1) * TPC], o_all[:, :, :NCTX]], axis=2)
        maps.append({"xT": xT_maps[k], "oT": np.ascontiguousarray(oT), "vec": vec, "w_o": I["mla_w_o"][0], "rw": rw})
    return maps


EPC = 2
FF = 1408
NFC = FF // 128
CAP_L = 2 * NLAT // 16
CAP_C = 2 * NCTX // 16
BIG = 1048576.0
NBIS = 28


def build_moe(with_ctx):
    NTOK = CAP_L + (CAP_C if with_ctx else 0)
    nc = bass.Bass("TRN2", target_bir_lowering=False)
    f_d = _din(nc, "f", [NALL if with_ctx else NLAT, D], BF16)
    aff_l = _din(nc, "aff_l", [EPC, 128, 128])
    tok_l = _din(nc, "tok_l", [128, 128], I32)
    if with_ctx:
        aff_c = _din(nc, "aff_c", [EPC, 128, 2])
        tok_c = _din(nc, "tok_c", [128, 2], I32)
    cst = _din(nc, "cst", [3, 128, 128])
    wg_d = _din(nc, "wg", [EPC, NFC, 128, 16, 128])
    wu_d = _din(nc, "wu", [EPC, NFC, 128, 16, 128])
    wd_d = _din(nc, "wd", [EPC, FF, D])
    y_o = _dout(nc, "y", [EPC, NTOK, D], BF16)
    lst_t = {}
    for e_ in range(EPC):
        lst_t[(e_, 0)] = _dout(nc, f"lst_l{e_}", [CAP_L, 2], I32)
        if with_ctx:
            lst_t[(e_, CAP_L)] = _dout(nc, f"lst_c{e_}", [CAP_C, 2], I32)
    inv_l = _dout(nc, "inv_l", [EPC, 128, 128], I32)
    if with_ctx:
        inv_c = _dout(nc, "inv_c", [EPC, 128, 2], I32)
    kb = KB(nc)
    kb.alloc_banks(8)
    ones_b, ones_f = load_consts(kb, nc)
    ident = kb.sb("ident", [128, 128], F32)
    identb = kb.sb("identb", [128, 128], BF16)
    U = kb.sb("U", [128, 128], F32)
    Ls = kb.sb("Ls", [128, 128], F32)
    kb.dma("sp", lambda e: e.dma_start(out=ident[:], in_=cst[0]), "cst", writes=["ident"])
    kb.dma("sp", lambda e: e.dma_start(out=U[:], in_=cst[1]), "cst", writes=["U"])
    kb.dma("sp", lambda e: e.dma_start(out=Ls[:], in_=cst[2]), "cst", writes=["Ls"])
    kb.op("dve", lambda e: e.tensor_copy(out=identb[:], in_=ident[:]), reads=["ident"], writes=["identb"])
    probs = []
    for e_ in range(EPC):
        probs.append((e_, 128, CAP_L, aff_l, tok_l, inv_l, 0))
    if with_ctx:
        for e_ in range(EPC):
            probs.append((e_, 2, CAP_C, aff_c, tok_c, inv_c, CAP_L))
    NP = len(probs)
    aff = [kb.sb(f"aff{i}", [128, p[1]], F32) for i, p in enumerate(probs)]
    tok = [kb.sb(f"tok{i}", [128, p[1]], I32) for i, p in enumerate(probs)]
    scr = [kb.sb(f"scr{i}", [128, p[1]], F32) for i, p in enumerate(probs)]
    msk = [kb.sb(f"msk{i}", [128, p[1]], F32) for i, p in enumerate(probs)]
    posf = [kb.sb(f"posf{i}", [128, p[1]], F32) for i, p in enumerate(probs)]
    invt = [kb.sb(f"invt{i}", [128, p[1]], I32) for i, p in enumerate(probs)]
    pair = [kb.sb(f"pair{i}", [128, p[1], 2], I32) for i, p in enumerate(probs)]
    mT = [kb.sb(f"mT{i}", [p[1], 128], F32) for i, p in enumerate(probs)]
    lo = kb.sb("lo", [128, NP], F32)
    mid = kb.sb("mid", [128, NP], F32)
    cnt = kb.sb("cnt", [128, NP], F32)
    ind = kb.sb("ind", [128, NP], F32)
    capt = kb.sb("capt", [128, NP], F32)
    totp = kb.sb("totp", [128, NP], F32)
    offp = kb.sb("offp", [128, NP], F32)
    for i, p in enumerate(probs):
        kb.dma("sp", lambda e, i=i, p=p: e.dma_start(out=aff[i][:], in_=p[3][p[0]]), f"aff{i}", writes=[f"aff{i}"])
        kb.dma("sp", lambda e, i=i, p=p: e.dma_start(out=tok[i][:], in_=p[4]), f"tok{i}", writes=[f"tok{i}"])
        kb.op("dve", lambda e, i=i, p=p: e.memset(capt[:, i:i + 1], p[2] - 0.5), writes=["capt"])
    kb.op("dve", lambda e: e.memset(lo[:], 0.0), writes=["lo"])
    for it in range(NBIS):
        w = 2.0 ** -(it + 1)
        kb.op("dve", lambda e, w=w: e.tensor_scalar(out=mid[:], in0=lo[:], scalar1=w, scalar2=None, op0=ALU.add), reads=["lo"], writes=["mid"])
        kb.op("dve", lambda e: e.memset(cnt[:], 0.0), writes=["cnt"])
        for i, p in enumerate(probs):
            kb.op("dve", lambda e, i=i: e.tensor_scalar(out=scr[i][:], in0=aff[i][:], scalar1=mid[:, i:i + 1], scalar2=0.0, op0=ALU.is_ge, op1=ALU.add,
                                                        accum_out=cnt[:, i:i + 1]), reads=[f"aff{i}", "mid", "cnt"], writes=[f"scr{i}", "cnt"])
        ps, pk = kb.bank()
        kb.op("pe", lambda e, ps=ps: e.matmul(ps[:, :NP], lhsT=ones_f[:], rhs=cnt[:], start=True, stop=True), reads=["ones_f", "cnt"], writes=[pk])
        kb.op("dve", lambda e, ps=ps: e.tensor_tensor(out=ind[:], in0=ps[:, :NP], in1=capt[:], op=ALU.is_ge), reads=[pk, "capt"], writes=["ind"])
        kb.op("dve", lambda e, w=w: e.scalar_tensor_tensor(out=lo[:], in0=ind[:], scalar=w, in1=lo[:], op0=ALU.mult, op1=ALU.add),
              reads=["ind", "lo"], writes=["lo"])
    kb.op("dve", lambda e: e.memset(totp[:], 0.0), writes=["totp"])
    for i, p in enumerate(probs):
        e_, J, cap, _, _, inv_d, rb = p
        kb.op("dve", lambda e, i=i: e.tensor_scalar(out=msk[i][:], in0=aff[i][:], scalar1=lo[:, i:i + 1], scalar2=0.0, op0=ALU.is_ge, op1=ALU.add,
                                                    accum_out=totp[:, i:i + 1]), reads=[f"aff{i}", "lo", "totp"], writes=[f"msk{i}", "totp"])
        ps, pk = kb.bank()
        kb.op("pe", lambda e, i=i, J=J, ps=ps: e.matmul(ps[:J, :128], lhsT=msk[i][:, :J], rhs=ident[:], start=True, stop=True),
              reads=[f"msk{i}", "ident"], writes=[pk])
        kb.op("act", lambda e, i=i, J=J, ps=ps: e.activation(out=mT[i][:J, :], in_=ps[:J, :128], func=AF.Copy), reads=[pk], writes=[f"mT{i}"])
        ps2, pk2 = kb.bank()
        kb.op("pe", lambda e, i=i, J=J, ps2=ps2: e.matmul(ps2[:, :J], lhsT=mT[i][:J, :], rhs=U[:J, :J], start=True, stop=True),
              reads=[f"mT{i}", "U"], writes=[pk2])
        ps3, pk3 = kb.bank()
        kb.op("pe", lambda e, i=i, ps3=ps3: e.matmul(ps3[:, :1], lhsT=Ls[:], rhs=totp[:, i:i + 1], start=True, stop=True),
              reads=["Ls", "totp"], writes=[pk3])
        kb.op("act", lambda e, i=i, ps3=ps3: e.activation(out=offp[:, i:i + 1], in_=ps3[:, :1], func=AF.Copy), reads=[pk3], writes=["offp"])
        kb.op("dve", lambda e, i=i, J=J, ps2=ps2: e.tensor_scalar(out=posf[i][:], in0=ps2[:, :J], scalar1=offp[:, i:i + 1], scalar2=-(1.0 + BIG),
                                                                  op0=ALU.add, op1=ALU.add), reads=[pk2, "offp"], writes=[f"posf{i}"])
        kb.op("dve", lambda e, i=i: e.tensor_tensor(out=posf[i][:], in0=posf[i][:], in1=msk[i][:], op=ALU.mult), reads=[f"posf{i}", f"msk{i}"], writes=[f"posf{i}"])
        kb.op("dve", lambda e, i=i: e.tensor_scalar(out=invt[i][:], in0=posf[i][:], scalar1=BIG, scalar2=None, op0=ALU.add), reads=[f"posf{i}"], writes=[f"invt{i}"])
        kb.dma("sp", lambda e, i=i, e_=e_, inv_d=inv_d: e.dma_start(out=inv_d[e_], in_=invt[i][:]), f"invo{i}", reads=[f"invt{i}"])
        kb.op("dve", lambda e, i=i: e.tensor_copy(out=pair[i][:, :, 0], in_=tok[i][:]), reads=[f"tok{i}"], writes=[f"pair{i}"])
        kb.op("dve", lambda e, i=i: e.tensor_copy(out=pair[i][:, :, 1], in_=aff[i][:].bitcast(I32)), reads=[f"aff{i}", f"pair{i}"], writes=[f"pair{i}"])
        for j in range(J):
            kb.dma("pool", lambda e, i=i, j=j, e_=e_, rb=rb, cap=cap: e.indirect_dma_start(
                out=lst_t[(e_, rb)][:, :], out_offset=bass.IndirectOffsetOnAxis(ap=invt[i][:, j:j + 1], axis=0), in_=pair[i][:, j, :], in_offset=None,
                bounds_check=breg(e, cap - 1), oob_is_err=False), f"sc{i}", reads=[f"pair{i}", f"invt{i}"], wadd=[f"lst{i}"])
    lsb = [kb.sb(f"lsb{e_}", [128, 17, 2], I32) for e_ in range(EPC)]
    xs_tok = [kb.sb(f"xstok{i}", [128, D], BF16) for i in range(2)]
    xsT = kb.sb("xsT", [128, 16, NTOK], BF16)
    hidT = kb.sb("hidT", [128, NFC, NTOK], BF16)
    wgc = [kb.sb(f"wgc{i}", [128, 16, 128], BF16) for i in range(2)]
    wuc = [kb.sb(f"wuc{i}", [128, 16, 128], BF16) for i in range(2)]
    wdb = [kb.sb(f"wdb{i}", [128, NFC, 512], BF16) for i in range(2)]
    sgt = [kb.sb(f"sgt{i}", [128, 512], F32) for i in range(2)]
    yst = [kb.sb(f"yst{i}", [128, 512], BF16) for i in range(2)]
    tiles = [(a * 128, 128) for a in range(16)] + ([(CAP_L, CAP_C)] if with_ctx else [])
    blocks = [(b * 512, 512) for b in range(4)] + ([(CAP_L, CAP_C)] if with_ctx else [])
    nw = 0
    for e_ in range(EPC):
        kb.dma("sp", lambda e, e_=e_: e.dma_start(out=lsb[e_][:, 0:16, :], in_=lst_t[(e_, 0)].rearrange("(a p) c -> p a c", p=128)),
               f"lsb{e_}", reads=[f"lst{e_}"], writes=[f"lsb{e_}"])
        if with_ctx:
            kb.dma("sp", lambda e, e_=e_: e.dma_start(out=lsb[e_][:CAP_C, 16, :], in_=lst_t[(e_, CAP_L)]),
                   f"lsb{e_}", reads=[f"lst{EPC + e_}"], writes=[f"lsb{e_}"])
        for a, (r0, R) in enumerate(tiles):
            xt = xs_tok[a % 2]
            kb.dma("pool", lambda e, e_=e_, a=a, R=R, xt=xt: e.indirect_dma_start(
                out=xt[:R, :], out_offset=None, in_=f_d[:, :], in_offset=bass.IndirectOffsetOnAxis(ap=lsb[e_][:R, a, 0:1], axis=0)),
                f"xstok{a % 2}", reads=[f"lsb{e_}"], writes=[f"xstok{a % 2}"])
            for g4 in range(4):
                ps, pk = kb.bank()
                for q in range(4):
                    kc = g4 * 4 + q
                    kb.op("pe", lambda e, kc=kc, q=q, R=R, xt=xt, ps=ps: e.matmul(ps[:, q * 128:q * 128 + R], lhsT=xt[:R, kc * 128:(kc + 1) * 128], rhs=identb[:R, :R],
                                                                                 start=True, stop=True), reads=[f"xstok{a % 2}", "identb"], writes=[pk])
                eng = "act" if g4 % 2 == 0 else "dve"
                src = lambda ps=ps, R=R: ps[:, :].rearrange("p (q c) -> p q c", q=4)[:, :, :R]
                if eng == "act":
                    kb.op("act", lambda e, g4=g4, r0=r0, R=R, src=src: e.activation(out=xsT[:, g4 * 4:g4 * 4 + 4, r0:r0 + R], in_=src(), func=AF.Copy),
                          reads=[pk], writes=["xsT"])
                else:
                    kb.op("dve", lambda e, g4=g4, r0=r0, R=R, src=src: e.tensor_copy(out=xsT[:, g4 * 4:g4 * 4 + 4, r0:r0 + R], in_=src()),
                          reads=[pk], writes=["xsT"])
        for fc in range(NFC):
            wb = nw % 2
            nw += 1
            kb.dma("pool", lambda e, e_=e_, fc=fc, wb=wb: e.dma_start(out=wgc[wb][:], in_=wg_d[e_, fc]), f"wgc{wb}", writes=[f"wgc{wb}"])
            kb.dma("pool", lambda e, e_=e_, fc=fc, wb=wb: e.dma_start(out=wuc[wb][:], in_=wu_d[e_, fc]), f"wuc{wb}", writes=[f"wuc{wb}"])
            for bi, (c0, W) in enumerate(blocks):
                psg, pkg = kb.bank()
                psu, pku = kb.bank()
                for kc in range(16):
                    kb.op("pe", lambda e, kc=kc, wb=wb, c0=c0, W=W, psg=psg: e.matmul(psg[:, :W], lhsT=wgc[wb][:, kc, :], rhs=xsT[:, kc, c0:c0 + W],
                                                                                     start=(kc == 0), stop=(kc == 15)), reads=[f"wgc{wb}", "xsT"], writes=[pkg])
                for kc in range(16):
                    kb.op("pe", lambda e, kc=kc, wb=wb, c0=c0, W=W, psu=psu: e.matmul(psu[:, :W], lhsT=wuc[wb][:, kc, :], rhs=xsT[:, kc, c0:c0 + W],
                                                                                     start=(kc == 0), stop=(kc == 15)), reads=[f"wuc{wb}", "xsT"], writes=[pku])
                sb_ = bi % 2
                kb.op("act", lambda e, sb_=sb_, W=W, psg=psg: e.activation(out=sgt[sb_][:, :W], in_=psg[:, :W], func=AF.Silu), reads=[pkg], writes=[f"sgt{sb_}"])
                kb.op("dve", lambda e, sb_=sb_, fc=fc, c0=c0, W=W, psu=psu: e.tensor_tensor(out=hidT[:, fc, c0:c0 + W], in0=sgt[sb_][:, :W], in1=psu[:, :W], op=ALU.mult),
                      reads=[f"sgt{sb_}", pku], writes=[f"hidT{fc}"])
        for nb in range(4):
            wb = nb % 2
            kb.dma("pool", lambda e, e_=e_, nb=nb, wb=wb: e.dma_start(out=wdb[wb][:], in_=wd_d[e_, :, nb * 512:(nb + 1) * 512].rearrange("(fc p) n -> p fc n", p=128)),
                   f"wdb{wb}", writes=[f"wdb{wb}"])
            for a, (r0, R) in enumerate(tiles):
                ps, pk = kb.bank()
                for fc in range(NFC):
                    kb.op("pe", lambda e, fc=fc, wb=wb, r0=r0, R=R, ps=ps: e.matmul(ps[:R, :], lhsT=hidT[:, fc, r0:r0 + R], rhs=wdb[wb][:, fc, :],
                                                                                   start=(fc == 0), stop=(fc == NFC - 1)), reads=[f"hidT{fc}", f"wdb{wb}"], writes=[pk])
                yb = a % 2
                kb.op("dve", lambda e, e_=e_, a=a, R=R, yb=yb, ps=ps: e.tensor_scalar(out=yst[yb][:R, :], in0=ps[:R, :], scalar1=lsb[e_][:R, a, 1:2].bitcast(F32), scalar2=None,
                                                                                     op0=ALU.mult), reads=[pk, f"lsb{e_}"], writes=[f"yst{yb}"])
                kb.dma("sp", lambda e, e_=e_, r0=r0, R=R, nb=nb, yb=yb: e.dma_start(out=y_o[e_, r0:r0 + R, nb * 512:(nb + 1) * 512], in_=yst[yb][:R, :]),
                       f"yst{yb}", reads=[f"yst{yb}"])
    kb.finish()
    return nc


def moe_consts():
    ident = np.eye(128, dtype=np.float32)
    U = np.triu(np.ones((128, 128), np.float32))
    Ls = np.triu(np.ones((128, 128), np.float32), 1)
    return np.stack([ident, U, Ls])


def prep_moe(I, layer, f_lat, aff_lat, f_ctx=None, aff_ctx=None):
    with_ctx = f_ctx is not None
    f_all = np.concatenate([f_lat, f_ctx], axis=0) if with_ctx else f_lat
    tok_l = np.arange(NLAT, dtype=np.int32).reshape(128, 128)
    tok_c = (NLAT + np.arange(NCTX, dtype=np.int32)).reshape(128, 2)
    cst = moe_consts()
    wg = I["moe_w_gate"][layer].reshape(16, 16, 128, NFC, 128).transpose(0, 3, 2, 1, 4)
    wu = I["moe_w_up"][layer].reshape(16, 16, 128, NFC, 128).transpose(0, 3, 2, 1, 4)
    maps = []
    for k in range(NCORES):
        es = slice(k * EPC, (k + 1) * EPC)
        m = {"f": f_all, "aff_l": np.ascontiguousarray(aff_lat[:, es].T).reshape(EPC, 128, 128), "tok_l": tok_l, "cst": cst,
             "wg": np.ascontiguousarray(wg[es]), "wu": np.ascontiguousarray(wu[es]), "wd": np.ascontiguousarray(I["moe_w_down"][layer][es])}
        if with_ctx:
            m["aff_c"] = np.ascontiguousarray(aff_ctx[:, es].T).reshape(EPC, 128, 2)
            m["tok_c"] = tok_c
        maps.append(m)
    return maps


NGB = 6


def combine_setup(kb, nc, ntiles, with_ctx):
    T = {}
    T["y"] = [_din(nc, f"y{e_}", [CAP_L, D], BF16) for e_ in range(16)]
    T["yc"] = [_din(nc, f"yc{e_}", [CAP_C, D], BF16) for e_ in range(16)] if with_ctx else None
    T["inv_d"] = _din(nc, "inv", [128, ntiles, 16], I32)
    T["inv"] = kb.sb("inv", [128, ntiles, 16], I32)
    T["acc"] = kb.sb("acc", [128, D], F32)
    T["g"] = [kb.sb(f"gth{i}", [128, D], BF16) for i in range(NGB)]
    T["ident"] = kb.sb("ident", [128, 128], F32)
    T["ng"] = 0
    kb.dma("sp", lambda e: e.dma_start(out=T["inv"][:], in_=T["inv_d"]), "inv", writes=["inv"])
    return T


def combine_block(kb, T, xb, vec, gcol, tiles):
    acc, ident = T["acc"], T["ident"]
    for (ti, col0, is_ctx) in tiles:
        ys, cap = (T["yc"], CAP_C) if is_ctx else (T["y"], CAP_L)
        for e_ in range(16):
            gi = T["ng"] % NGB
            T["ng"] += 1
            g = T["g"][gi]
            kb.op("pool", lambda e, g=g: e.memset(g[:], 0.0), writes=[f"gth{gi}"])
            kb.dma("pool", lambda e, g=g, e_=e_, ti=ti, ys=ys, cap=cap: e.indirect_dma_start(
                out=g[:, :], out_offset=None, in_=ys[e_][:, :], in_offset=bass.IndirectOffsetOnAxis(ap=T["inv"][:, ti, e_:e_ + 1], axis=0),
                bounds_check=breg(e, cap - 1), oob_is_err=False), f"gth{gi}", reads=["inv"], writes=[f"gth{gi}"])
            if e_ == 0:
                kb.op("dve", lambda e, g=g: e.tensor_copy(out=acc[:], in_=g[:]), reads=[f"gth{gi}"], writes=["acc"])
            else:
                kb.op("dve", lambda e, g=g: e.tensor_tensor(out=acc[:], in0=acc[:], in1=g[:], op=ALU.add), reads=[f"gth{gi}", "acc"], writes=["acc"])
        for g4 in range(4):
            ps, pk = kb.bank()
            for q in range(4):
                kc = g4 * 4 + q
                kb.op("pe", lambda e, kc=kc, q=q, ps=ps: e.matmul(ps[:, q * 128:(q + 1) * 128], lhsT=acc[:, kc * 128:(kc + 1) * 128], rhs=ident[:],
                                                                 start=True, stop=True), reads=["acc", "ident"], writes=[pk])
            for q in range(4):
                kc = g4 * 4 + q
                kb.op("dve", lambda e, kc=kc, q=q, ps=ps, col0=col0, gc=gcol + (1 if is_ctx else 0): e.scalar_tensor_tensor(
                    out=xb[:, kc, col0:col0 + 128], in0=ps[:, q * 128:(q + 1) * 128], scalar=vec[:, kc, gc:gc + 1], in1=xb[:, kc, col0:col0 + 128],
                    op0=ALU.mult, op1=ALU.add), reads=[pk, "vec", f"xb{kc}"], writes=[f"xb{kc}"])


def build_comb_qkv1(dbg_p1=True, dbg_p2=True):
    nc = bass.Bass("TRN2", target_bir_lowering=False)
    x1T = _din(nc, "x1T", [D, NT1])
    vecd = _din(nc, "vec", [128, 16, 7])
    gaind = _din(nc, "gains", [128, 2])
    cst = _din(nc, "cst", [128, 128])
    w_qkv = _din(nc, "w_qkv", [D, 3 * D])
    x2_o = _dout(nc, "x2T", [D, NT1])
    q_o = _dout(nc, "q", [128, 16, NT1], BF16)
    k_o = _dout(nc, "k", [128, 16, NT1], BF16)
    v_o = _dout(nc, "v", [NT1, D], BF16)
    kb = KB(nc)
    kb.alloc_banks(8)
    ones_b, ones_f = load_consts(kb, nc)
    T = combine_setup(kb, nc, 18, True)
    kb.dma("sp", lambda e: e.dma_start(out=T["ident"][:], in_=cst), "cst", writes=["ident"])
    vec = kb.sb("vec", [128, 16, 7], F32)
    gains = kb.sb("gains", [128, 2], F32)
    A = kb.sb("A", [128, 16, 2], F32)
    B = kb.sb("B", [128, 16, 2], F32)
    kb.dma("sp", lambda e: e.dma_start(out=vec[:], in_=vecd), "vec", writes=["vec"])
    kb.dma("sp", lambda e: e.dma_start(out=gains[:], in_=gaind), "gains", writes=["gains"])
    make_AB(kb, vec, A, B, 2, [3, 5], [4, 6], 2)
    xb = kb.sb("xb", [128, 16, 256], F32)
    hs = kb.sb("hs", [128, 16, 256], BF16)
    hT = kb.sb("hT", [128, 16, NT1], BF16)
    rs = kb.sb("rs", [128, 256], F32)
    tmp = [kb.sb(f"tmp{i}", [128, 256], F32) for i in range(2)]
    blocks = [(i * 256, 256, 0) for i in range(8)] + [(2048, 256, 1)]

    def phase1(bi, c0, W, s):
        kb.dma("sp", lambda e: e.dma_start(out=xb[:, :, :W], in_=x1T[:, c0:c0 + W].rearrange("(kc p) n -> p kc n", p=128)),
               "xb", writes=K("xb", range(16)))
        combine_block(kb, T, xb, vec, 0, [(bi * 2, 0, s == 1), (bi * 2 + 1, 128, s == 1)])
        kb.dma("sp", lambda e: e.dma_start(out=x2_o[:, c0:c0 + W].rearrange("(kc p) n -> p kc n", p=128), in_=xb[:, :, :W]),
               "x2o", reads=K("xb", range(16)))
        norm_mod_block(kb, xb, K("xb", range(16)), hs, rs, tmp, A, B, s, W, ones_b)
        for kc in range(16):
            kb.op("pool", lambda e, kc=kc: e.tensor_copy(out=hT[:, kc, c0:c0 + W], in_=hs[:, kc, :W]), reads=[f"hs{kc}"], writes=["hT"])

    for bi, blk in enumerate(blocks):
        if dbg_p1:
            phase1(bi, *blk)

    wch = [kb.sb(f"wch{i}", [128, 16, 512], BF16) for i in range(2)]
    sqa = [kb.sb(f"sqa{i}", [128, 256], BF16) for i in range(2)]
    rh = [kb.sb(f"rh{i}", [128, 256], F32) for i in range(2)]
    stn = [kb.sb(f"stn{i}", [128, 4, 256], BF16) for i in range(2)]
    vst = [kb.sb(f"vst{i}", [128, 512], BF16) for i in range(2)]
    nst = [0]

    def qk_chunk(ci, which, out_d, gcol):
        wb = ci % 2
        for kc in range(16):
            kb.dma("pool", lambda e, kc=kc: e.dma_start(out=wch[wb][:, kc, :], in_=w_qkv[kc * 128:(kc + 1) * 128, which * D + (ci % 4) * 512:which * D + (ci % 4 + 1) * 512]),
                   f"wch{wb}", writes=[f"wch{wb}"])
        units = []
        for (c0, W, s) in blocks:
            sb_ = nst[0] % 2
            nst[0] += 1
            for hh in range(4):
                units.append((c0, W, hh, sb_))

        def proj(u):
            c0, W, hh, sb_ = u
            ps, pk = kb.bank()
            for kc in range(16):
                kb.op("pe", lambda e, kc=kc: e.matmul(ps[:, :W], lhsT=wch[wb][:, kc, hh * 128:(hh + 1) * 128], rhs=hT[:, kc, c0:c0 + W],
                                                      start=(kc == 0), stop=(kc == 15)), reads=[f"wch{wb}", "hT"], writes=[pk])
            return ps, pk

        def norm(u, ps, pk):
            c0, W, hh, sb_ = u
            i2 = hh % 2
            kb.op("act", lambda e: e.activation(out=sqa[i2][:, :W], in_=ps[:, :W], func=AF.Square), reads=[pk], writes=[f"sqa{i2}"])
            pq, pkq = kb.bank()
            kb.op("pe", lambda e: e.matmul(pq[:, :W], lhsT=ones_b[:], rhs=sqa[i2][:, :W], start=True, stop=True),
                  reads=[f"sqa{i2}", "ones_b"], writes=[pkq])
            rstd_from_psum(kb, pq, pkq, rh[i2], f"rh{i2}", W, 128)
            kb.op("dve", lambda e: e.scalar_tensor_tensor(out=stn[sb_][:, hh, :W], in0=ps[:, :W], scalar=gains[:, gcol:gcol + 1],
                                                          in1=rh[i2][:, :W], op0=ALU.mult, op1=ALU.mult),
                  reads=[pk, "gains", f"rh{i2}"], writes=[f"stn{sb_}"])
            if hh == 3:
                g4 = ci % 4
                kb.dma("sp", lambda e: e.dma_start(out=out_d[:, g4 * 4:g4 * 4 + 4, c0:c0 + W], in_=stn[sb_][:, :, :W]),
                       f"stn{sb_}", reads=[f"stn{sb_}"])

        nxt = proj(units[0])
        for ui, u in enumerate(units):
            cur = nxt
            if ui + 1 < len(units):
                nxt = proj(units[ui + 1])
            norm(u, *cur)

    def v_chunk(ci):
        wb = ci % 2
        for kc in range(16):
            kb.dma("pool", lambda e, kc=kc: e.dma_start(out=wch[wb][:, kc, :], in_=w_qkv[kc * 128:(kc + 1) * 128, 2 * D + (ci % 4) * 512:2 * D + (ci % 4 + 1) * 512]),
                   f"wch{wb}", writes=[f"wch{wb}"])
        for tt in range(NT1 // 128):
            ps, pk = kb.bank()
            for kc in range(16):
                kb.op("pe", lambda e, kc=kc, tt=tt, ps=ps: e.matmul(ps[:, :], lhsT=hT[:, kc, tt * 128:(tt + 1) * 128], rhs=wch[wb][:, kc, :],
                                                                    start=(kc == 0), stop=(kc == 15)), reads=[f"wch{wb}", "hT"], writes=[pk])
            vb = tt % 2
            if tt % 2 == 0:
                kb.op("act", lambda e, vb=vb, ps=ps: e.activation(out=vst[vb][:], in_=ps[:, :], func=AF.Copy), reads=[pk], writes=[f"vst{vb}"])
            else:
                kb.op("dve", lambda e, vb=vb, ps=ps: e.tensor_copy(out=vst[vb][:], in_=ps[:, :]), reads=[pk], writes=[f"vst{vb}"])
            nb = ci % 4
            kb.dma("sp", lambda e, tt=tt, vb=vb, nb=nb: e.dma_start(out=v_o[tt * 128:(tt + 1) * 128, nb * 512:(nb + 1) * 512], in_=vst[vb][:]),
                   f"vst{vb}", reads=[f"vst{vb}"])

    ci = 0
    if not dbg_p2:
        kb.finish()
        return nc
    for g4 in range(4):
        qk_chunk(ci, 0, q_o, 0)
        ci += 1
    for g4 in range(4):
        qk_chunk(ci, 1, k_o, 1)
        ci += 1
    for g4 in range(4):
        v_chunk(ci)
        ci += 1
    kb.finish()
    return nc


def build_final():
    nc = bass.Bass("TRN2", target_bir_lowering=False)
    x1T = _din(nc, "x1T", [D, TPC])
    vecd = _din(nc, "vec", [128, 16, 1])
    cst = _din(nc, "cst", [128, 128])
    x3_o = _dout(nc, "x3T", [D, TPC])
    kb = KB(nc)
    kb.alloc_banks(8)
    T = combine_setup(kb, nc, 16, False)
    kb.dma("sp", lambda e: e.dma_start(out=T["ident"][:], in_=cst), "cst", writes=["ident"])
    vec = kb.sb("vec", [128, 16, 1], F32)
    kb.dma("sp", lambda e: e.dma_start(out=vec[:], in_=vecd), "vec", writes=["vec"])
    xb = kb.sb("xb", [128, 16, 256], F32)

    def blk(bi, c0, W):
        kb.dma("sp", lambda e: e.dma_start(out=xb[:, :, :W], in_=x1T[:, c0:c0 + W].rearrange("(kc p) n -> p kc n", p=128)),
               "xb", writes=K("xb", range(16)))
        combine_block(kb, T, xb, vec, 0, [(bi * 2, 0, False), (bi * 2 + 1, 128, False)])
        kb.dma("sp", lambda e: e.dma_start(out=x3_o[:, c0:c0 + W].rearrange("(kc p) n -> p kc n", p=128), in_=xb[:, :, :W]),
               "x3o", reads=K("xb", range(16)))

    for bi in range(8):
        blk(bi, bi * 256, 256)
    kb.finish()
    return nc


def gather_moe_outputs(res4, with_ctx):
    y = [np.asarray(res4[e_ // EPC]["y"])[e_ % EPC] for e_ in range(16)]
    inv_l = np.stack([np.asarray(res4[e_ // EPC]["inv_l"])[e_ % EPC].reshape(NLAT) for e_ in range(16)], axis=-1)
    inv_c = None
    if with_ctx:
        inv_c = np.stack([np.asarray(res4[e_ // EPC]["inv_c"])[e_ % EPC].reshape(NCTX) for e_ in range(16)], axis=-1)
    return y, inv_l, inv_c


def prep_comb_qkv1(I, mod, res3, res4):
    y, inv_l, inv_c = gather_moe_outputs(res4, True)
    ml0, mc0 = mod[0, 0], mod[0, 1]
    ml1, mc1 = mod[1, 0], mod[1, 1]
    vec = np.stack([pvec(ml0[5 * D:6 * D]), pvec(mc0[5 * D:6 * D]), pvec(I["norm_mix"][1]),
                    pvec(ml1[D:2 * D]), pvec(ml1[0:D]), pvec(mc1[D:2 * D]), pvec(mc1[0:D])], axis=-1).astype(np.float32)
    gains = np.stack([I["na_q_gain"][0], I["na_k_gain"][0]], axis=-1).astype(np.float32)
    maps = []
    for k in range(NCORES):
        inv = np.concatenate([inv_l[k * TPC:(k + 1) * TPC], inv_c], axis=0).reshape(18, 128, 16).transpose(1, 0, 2)
        m = {"x1T": np.asarray(res3[k]["x1T"]), "vec": vec, "gains": gains, "cst": np.eye(128, dtype=np.float32),
             "w_qkv": I["na_w_qkv"][0], "inv": np.ascontiguousarray(inv)}
        for e_ in range(16):
            m[f"y{e_}"] = np.ascontiguousarray(y[e_][:CAP_L])
            m[f"yc{e_}"] = np.ascontiguousarray(y[e_][CAP_L:])
        maps.append(m)
    return maps


GW = 64
ROWS = NLAT // GW
RPC = ROWS // NCORES
NA_SPEC = [0, 1, 2, 3, 29, 30, 31]
SCALE1 = 128 ** -0.5
NEG = -30000.0


def build_na():
    nc = bass.Bass("TRN2", target_bir_lowering=False)
    q_d = _din(nc, "q", [128, 16, NT1], BF16)
    k_d = _din(nc, "k", [128, 16, NT1], BF16)
    v_d = _din(nc, "v", [NT1, D], BF16)
    ks_d = _din(nc, "kspec", [len(NA_SPEC), 128, 16, 512], BF16)
    vs_d = _din(nc, "vspec", [len(NA_SPEC), 512, D], BF16)
    bi_d = _din(nc, "bias_int", [128, 4, 16, 64])
    bs_d = _din(nc, "bias_spec", [len(NA_SPEC), 128, 4, 16, 64])
    o_d = _dout(nc, "oT", [128, 16, TPC], BF16)
    kb = KB(nc)
    ones_b, ones_f = load_consts(kb, nc)
    st = [kb.ps(f"st{i}", [128, 512], F32) for i in range(3)]
    oacc = [kb.ps(f"oacc{i}", [128, 512], F32) for i in range(2)]
    lacc = [kb.ps(f"lacc{i}", [128, 512], F32) for i in range(2)]
    Kc = kb.sb("Kc", [128, 16, NCTX], BF16)
    Vc = kb.sb("Vc", [128, 2, D], BF16)
    bint = kb.sb("bint", [128, 4, 16, 64], F32)
    bsp = kb.sb("bsp", [128, 4, 16, 64], F32)
    kt_sb = [kb.sb(f"ktile{i}", [128, 16, 512], BF16) for i in range(2)]
    vt_sb = [kb.sb(f"vtile{i}", [128, 4, D], BF16) for i in range(2)]
    qrow = [kb.sb(f"qrow{i}", [128, 16, 64], BF16) for i in range(2)]
    tt_ = [kb.sb(f"tt{i}", [128, 512], F32) for i in range(2)]
    pT = [kb.sb(f"pT{i}", [128, 512], BF16) for i in range(3)]
    rcp = [kb.sb(f"rcp{i}", [128, 512], F32) for i in range(2)]
    ost = [kb.sb(f"ost{i}", [128, 16, 256], BF16) for i in range(2)]
    kb.dma("sp", lambda e: e.dma_start(out=Kc[:], in_=k_d[:, :, TPC:]), "Kc", writes=["Kc"])
    kb.dma("sp", lambda e: e.dma_start(out=Vc[:], in_=v_d[TPC:, :].rearrange("(kt p) n -> p kt n", p=128)), "Vc", writes=["Vc"])
    kb.dma("sp", lambda e: e.dma_start(out=bint[:], in_=bi_d), "bint", writes=["bint"])
    cnt = [0, 0]

    def do_row(lr):
        b = lr % 2
        kb.dma("sp", lambda e: e.dma_start(out=qrow[b][:], in_=q_d[:, :, lr * 64:(lr + 1) * 64]), f"qrow{b}", writes=[f"qrow{b}"])
        if lr in NA_SPEC:
            si = NA_SPEC.index(lr)
            kb.dma("sp", lambda e: e.dma_start(out=kt_sb[b][:], in_=ks_d[si]), f"ktile{b}", writes=[f"ktile{b}"])
            kb.dma("sp", lambda e: e.dma_start(out=vt_sb[b][:], in_=vs_d[si].rearrange("(kt p) n -> p kt n", p=128)), f"vtile{b}", writes=[f"vtile{b}"])
            kb.dma("sp", lambda e: e.dma_start(out=bsp[:], in_=bs_d[si]), "bsp", writes=["bsp"])
            bias, bkey = bsp, "bsp"
        else:
            c0 = (lr - 4) * 64
            kb.dma("sp", lambda e: e.dma_start(out=kt_sb[b][:], in_=k_d[:, :, c0:c0 + 512]), f"ktile{b}", writes=[f"ktile{b}"])
            kb.dma("sp", lambda e: e.dma_start(out=vt_sb[b][:], in_=v_d[c0:c0 + 512, :].rearrange("(kt p) n -> p kt n", p=128)), f"vtile{b}", writes=[f"vtile{b}"])
            bias, bkey = bint, "bint"
        so = (lr // 4) % 2

        def do_hg(hg):
            def qk(kt):
                si_ = cnt[0] % 3
                cnt[0] += 1
                for hh in range(8):
                    h = hg * 8 + hh
                    if kt < 4:
                        kb.op("pe", lambda e, h=h, hh=hh, kt=kt, si_=si_: e.matmul(st[si_][:, hh * 64:(hh + 1) * 64], lhsT=kt_sb[b][:, h, kt * 128:(kt + 1) * 128], rhs=qrow[b][:, h, :],
                                                                                  start=True, stop=True), reads=[f"ktile{b}", f"qrow{b}"], writes=[f"st{si_}"])
                    else:
                        kb.op("pe", lambda e, h=h, hh=hh, kt=kt, si_=si_: e.matmul(st[si_][:, hh * 64:(hh + 1) * 64], lhsT=Kc[:, h, (kt - 4) * 128:(kt - 3) * 128], rhs=qrow[b][:, h, :],
                                                                                  start=True, stop=True), reads=["Kc", f"qrow{b}"], writes=[f"st{si_}"])
                return si_

            nxt = qk(0)
            for kt in range(6):
                si_ = nxt
                if kt + 1 < 6:
                    nxt = qk(kt + 1)
                if kt < 4:
                    ti = cnt[1] % 2
                    cnt[1] += 1
                    kb.op("dve", lambda e, kt=kt, si_=si_, ti=ti: e.scalar_tensor_tensor(
                        out=tt_[ti][:], in0=st[si_][:], scalar=SCALE1, in1=bias[:, kt, hg * 8:(hg + 1) * 8, :].rearrange("p h q -> p (h q)"),
                        op0=ALU.mult, op1=ALU.add), reads=[f"st{si_}", bkey], writes=[f"tt{ti}"])
                    kb.op("act", lambda e, si_=si_, ti=ti: e.activation(out=pT[si_][:], in_=tt_[ti][:], func=AF.Exp), reads=[f"tt{ti}"], writes=[f"pT{si_}"])
                else:
                    kb.op("act", lambda e, si_=si_: e.activation(out=pT[si_][:], in_=st[si_][:], func=AF.Exp, scale=SCALE1), reads=[f"st{si_}"], writes=[f"pT{si_}"])
                for hh in range(8):
                    h = hg * 8 + hh
                    if kt < 4:
                        kb.op("pe", lambda e, h=h, hh=hh, kt=kt, si_=si_: e.matmul(oacc[hg][:, hh * 64:(hh + 1) * 64], lhsT=vt_sb[b][:, kt, h * 128:(h + 1) * 128], rhs=pT[si_][:, hh * 64:(hh + 1) * 64],
                                                                                  start=(kt == 0 and hh == 0), stop=(kt == 5), skip_group_check=True), reads=[f"vtile{b}", f"pT{si_}"], writes=[f"oacc{hg}"])
                    else:
                        kb.op("pe", lambda e, h=h, hh=hh, kt=kt, si_=si_: e.matmul(oacc[hg][:, hh * 64:(hh + 1) * 64], lhsT=Vc[:, kt - 4, h * 128:(h + 1) * 128], rhs=pT[si_][:, hh * 64:(hh + 1) * 64],
                                                                                  start=False, stop=(kt == 5), skip_group_check=True), reads=["Vc", f"pT{si_}"], writes=[f"oacc{hg}"])
                kb.op("pe", lambda e, kt=kt, si_=si_: e.matmul(lacc[hg][:], lhsT=ones_b[:], rhs=pT[si_][:], start=(kt == 0), stop=(kt == 5)),
                      reads=["ones_b", f"pT{si_}"], writes=[f"lacc{hg}"])
            kb.op("dve", lambda e: e.reciprocal(out=rcp[hg][:], in_=lacc[hg][:]), reads=[f"lacc{hg}"], writes=[f"rcp{hg}"])
            kb.op("dve", lambda e: e.tensor_tensor(out=ost[so][:, hg * 8:(hg + 1) * 8, (lr % 4) * 64:(lr % 4 + 1) * 64],
                                                   in0=oacc[hg][:].rearrange("p (h q) -> p h q", h=8), in1=rcp[hg][:].rearrange("p (h q) -> p h q", h=8), op=ALU.mult),
                  reads=[f"oacc{hg}", f"rcp{hg}"], writes=[f"ost{so}"])
        for hg_ in range(2):
            do_hg(hg_)
        if lr % 4 == 3:
            kb.dma("sp", lambda e: e.dma_start(out=o_d[:, :, (lr - 3) * 64:(lr + 1) * 64], in_=ost[so][:]), f"ost{so}", reads=[f"ost{so}"])

    for lr in range(RPC):
        do_row(lr)
    kb.finish()
    return nc


def na_bias_table(rpb, r):
    col = np.arange(GW)
    col_start = np.clip(col - 8, 0, GW - 16)
    mask = (col[None, :] >= col_start[:, None]) & (col[None, :] < col_start[:, None] + 16)
    dc_idx = np.clip(col[None, :] - col[:, None], -15, 15) + 15
    r0 = int(np.clip(r - 4, 0, ROWS - 8))
    dr = r0 + np.arange(8) - r
    b = rpb[:, dr + 7][:, :, dc_idx]
    b = np.where(mask[None, None], b, np.float32(NEG)).astype(np.float32)
    b = b.transpose(1, 3, 0, 2).reshape(4, 128, 16, GW)
    return np.ascontiguousarray(b.transpose(1, 0, 2, 3)), r0


def prep_na(I, res5):
    rpb = I["na_rpb"][0]
    kg = np.concatenate([np.asarray(res5[k]["k"])[:, :, :TPC] for k in range(NCORES)], axis=2)
    vg = np.concatenate([np.asarray(res5[k]["v"])[:TPC] for k in range(NCORES)], axis=0)
    bint, _ = na_bias_table(rpb, 100)
    maps = []
    for k in range(NCORES):
        ks, vs, bs = [], [], []
        for lr in NA_SPEC:
            b, r0 = na_bias_table(rpb, k * RPC + lr)
            bs.append(b)
            ks.append(kg[:, :, r0 * GW:(r0 + 8) * GW])
            vs.append(vg[r0 * GW:(r0 + 8) * GW])
        maps.append({"q": np.asarray(res5[k]["q"]), "k": np.asarray(res5[k]["k"]), "v": np.asarray(res5[k]["v"]),
                     "kspec": np.ascontiguousarray(np.stack(ks)), "vspec": np.ascontiguousarray(np.stack(vs)),
                     "bias_int": bint, "bias_spec": np.stack(bs)})
    return maps


def _run(nc, maps):
    r = run_bass_kernel_spmd(nc, maps, core_ids=list(range(NCORES)))
    return r.results


def prep_post1(I, mod, res5, res6):
    ml = mod[1, 0]
    vec = np.stack([pvec(ml[2 * D:3 * D]), pvec(ml[2 * D:3 * D]), pvec(I["norm_ffn"][1]),
                    pvec(ml[4 * D:5 * D]), pvec(ml[3 * D:4 * D]), pvec(ml[4 * D:5 * D]), pvec(ml[3 * D:4 * D])], axis=-1).astype(np.float32)
    rw = np.ascontiguousarray(I["router_w"][1].reshape(16, 128, 16).transpose(1, 0, 2))
    maps = []
    for k in range(NCORES):
        maps.append({"xT": np.ascontiguousarray(np.asarray(res5[k]["x2T"])[:, :TPC]), "oT": np.asarray(res6[k]["oT"]),
                     "vec": vec, "w_o": I["na_w_o"][0], "rw": rw})
    return maps


def prep_final(I, mod, res7, res8):
    y, inv_l, _ = gather_moe_outputs(res8, False)
    vec = pvec(mod[1, 0][5 * D:6 * D]).astype(np.float32)[:, :, None]
    maps = []
    for k in range(NCORES):
        inv = inv_l[k * TPC:(k + 1) * TPC].reshape(16, 128, 16).transpose(1, 0, 2)
        m = {"x1T": np.asarray(res7[k]["x1T"]), "vec": np.ascontiguousarray(vec), "cst": np.eye(128, dtype=np.float32),
             "inv": np.ascontiguousarray(inv)}
        for e_ in range(16):
            m[f"y{e_}"] = np.ascontiguousarray(y[e_])
        maps.append(m)
    return maps


def kernel(**inputs):
    I = {k: np.asarray(v) for k, v in inputs.items()}
    mod = run_mod(I)
    m1 = prep_qkv0(I, mod)
    res1 = _run(build_qkv0(), m1)
    res2 = _run(build_attn0(), prep_attn0(res1))
    del res1
    res3 = _run(build_post(True), prep_post0(I, mod, res2, [m["xT"] for m in m1]))
    del res2, m1
    fT = np.concatenate([np.asarray(res3[k]["fT"])[:, :TPC] for k in range(NCORES)], axis=1)
    f_lat = np.ascontiguousarray(fT.T)
    f_ctx = np.ascontiguousarray(np.asarray(res3[0]["fT"])[:, TPC:].T)
    aff_lat = np.concatenate([np.asarray(res3[k]["aff"])[:TPC] for k in range(NCORES)], axis=0)
    aff_ctx = np.asarray(res3[0]["aff"])[TPC:]
    res4 = _run(build_moe(True), prep_moe(I, 0, f_lat, aff_lat, f_ctx, aff_ctx))
    res5 = _run(build_comb_qkv1(), prep_comb_qkv1(I, mod, res3, res4))
    del res3, res4
    res6 = _run(build_na(), prep_na(I, res5))
    res7 = _run(build_post(False), prep_post1(I, mod, res5, res6))
    del res5, res6
    fT = np.concatenate([np.asarray(res7[k]["fT"]) for k in range(NCORES)], axis=1)
    f_lat = np.ascontiguousarray(fT.T)
    aff_lat = np.concatenate([np.asarray(res7[k]["aff"]) for k in range(NCORES)], axis=0)
    res8 = _run(build_moe(False), prep_moe(I, 1, f_lat, aff_lat))
    res9 = _run(build_final(), prep_final(I, mod, res7, res8))
    out = np.concatenate([np.asarray(res9[k]["x3T"]).T for k in range(NCORES)], axis=0)
    return np.ascontiguousarray(out[None].astype(np.float32))
```
